# Optimizing a Trainium2 kernel written in Bass

```python
import math
import jax
import jax.numpy as jnp
from jax import lax
import numpy as np

D_MODEL = 1024
BATCH = 4
SEQ = 4096
DEPTH = 2

GRID_W = 64
CTX_LEN = 256
EPS = 1e-6
NEG_INF = -1e30

A_HEADS = 4
A_DK = 128
A_DV = 128
A_WIDTH = A_HEADS * A_DV
CONV_K = 5
CHUNK = 64
B_HEADS = 4
B_DH = 64
B_WIDTH = B_HEADS * 2 * B_DH
ROPE_BASE = 10000.0
Q_BLOCK = 128
C_HEADS = 8
C_DH = 64
C_WIDTH = C_HEADS * C_DH
WIN_R = 8
WIN_C = 16
N_BRANCH = 3
BRANCH_W = 512
P_HEADS = 8
N_KEYS = 128
N_EXPERTS = N_KEYS * N_KEYS
P_DKH = 128
P_TOPK = 16
P_BLOCK = 128

SPLIT_SIZES = (A_HEADS * A_DK, A_HEADS * A_DK, A_WIDTH, A_WIDTH, 2 * A_HEADS, 2 * A_HEADS,
               B_HEADS * 2 * B_DH, B_HEADS * 2 * B_DH, B_WIDTH,
               C_WIDTH, C_WIDTH, C_WIDTH,
               N_BRANCH * D_MODEL)
IN_COLS = sum(SPLIT_SIZES)

kernel_name = 'hybrid_diffusion_trunk'


def rmsnorm(x, g):
    xf = x.astype(jnp.float32)
    y = xf * lax.rsqrt(jnp.mean(xf * xf, axis=-1, keepdims=True) + EPS)
    return (y * g.astype(jnp.float32)).astype(x.dtype)


def l2norm(x):
    return x * lax.rsqrt(jnp.sum(x * x, axis=-1, keepdims=True) + EPS)


def modulate(h, shift, scale):
    return h * (1 + scale) + shift


def split_cols(t):
    return jnp.split(t, [int(s) for s in np.cumsum(SPLIT_SIZES)[:-1]], axis=-1)


def dwconv_centred(x, w):
    ch = x.shape[-1]
    pad = CONV_K // 2
    return lax.conv_general_dilated(x, w[:, None, :].astype(x.dtype), window_strides=(1,),
                                    padding=[(pad, pad)], dimension_numbers=('NWC', 'WIO', 'NWC'),
                                    feature_group_count=ch)


def rope_2d(x, row_pos, col_pos):
    def rot(t, pos):
        half = t.shape[-1] // 2
        inv = 1.0 / (ROPE_BASE ** (jnp.arange(half, dtype=jnp.float32) / half))
        ang = pos.astype(jnp.float32)[:, None] * inv
        cos, sin = jnp.cos(ang), jnp.sin(ang)
        t1, t2 = t[..., :half], t[..., half:]
        return jnp.concatenate([t1 * cos - t2 * sin, t1 * sin + t2 * cos], axis=-1)
    d_axis = x.shape[-1] // 2
    return jnp.concatenate([rot(x[..., :d_axis], row_pos), rot(x[..., d_axis:], col_pos)], axis=-1).astype(x.dtype)


def gated_delta_chunked(q, k, v, beta, g, s0):
    Bn, H, L, DK = q.shape
    DV = v.shape[-1]
    n = L // CHUNK
    rs = lambda t: t.reshape(Bn, H, n, CHUNK, *t.shape[3:])
    q, k, v, beta, g = rs(q), rs(k), rs(v), rs(beta), rs(g)
    g = jnp.cumsum(g, axis=-1)
    kb = k * beta[..., None]
    vb = v * beta[..., None]
    tri_incl = jnp.tril(jnp.ones((CHUNK, CHUNK), bool))
    tri_strict = jnp.tril(jnp.ones((CHUNK, CHUNK), bool), -1)
    decay_mat = jnp.exp(jnp.where(tri_incl, g[..., :, None] - g[..., None, :], -jnp.inf))
    a_strict = jnp.where(tri_strict, jnp.einsum('bhnid,bhnjd->bhnij', kb, k) * decay_mat, 0.0)
    rhs = jnp.concatenate([vb, kb * jnp.exp(g)[..., None]], axis=-1)
    sol = jax.lax.linalg.triangular_solve(a_strict, rhs, left_side=True, lower=True, unit_diagonal=True)
    u, w = sol[..., :DV], sol[..., DV:]
    qk = jnp.where(tri_incl, jnp.einsum('bhnid,bhnjd->bhnij', q, k) * decay_mat, 0.0)
    g_last = g[..., -1]
    k_dec = k * jnp.exp(g_last[..., None] - g)[..., None]
    q_dec = q * jnp.exp(g)[..., None]

    def step(S, xs):
        qc, qkc, uc, wc, kdc, glc = xs
        v_new = uc - jnp.einsum('bhcd,bhde->bhce', wc, S)
        o = jnp.einsum('bhcd,bhde->bhce', qc, S) + jnp.einsum('bhij,bhje->bhie', qkc, v_new)
        S = S * jnp.exp(glc)[..., None, None] + jnp.einsum('bhcd,bhce->bhde', kdc, v_new)
        return S, o

    xs = tuple(jnp.moveaxis(t, 2, 0) for t in (q_dec, qk, u, w, k_dec, g_last))
    s_final, o = lax.scan(step, s0, xs)
    return jnp.moveaxis(o, 0, 2).reshape(Bn, H, L, DV), s_final


def gdn_prep(parts, conv_w, a_log, dt_bias):
    q, k, v, _, b, a = parts
    Bn, L, _ = q.shape
    qkv = jax.nn.silu(dwconv_centred(jnp.concatenate([q, k, v], axis=-1), conv_w)).astype(jnp.float32)
    q, k, v = jnp.split(qkv, 3, axis=-1)
    heads = lambda t, d: t.reshape(Bn, L, A_HEADS, d).transpose(0, 2, 1, 3)
    q = l2norm(heads(q, A_DK)) * (A_DK ** -0.5)
    k = l2norm(heads(k, A_DK))
    v = heads(v, A_DV)
    dirs = lambda t: t.astype(jnp.float32).reshape(Bn, L, 2, A_HEADS).transpose(2, 0, 3, 1)
    beta = jax.nn.sigmoid(dirs(b))
    g = -jnp.exp(a_log.astype(jnp.float32))[:, None, :, None] * jax.nn.softplus(
        dirs(a) + dt_bias.astype(jnp.float32)[:, None, :, None])
    return q, k, v, beta, g


def gdn_out(o, z, norm_g):
    Bn, H, L, DV = o.shape
    o = rmsnorm(o.transpose(0, 2, 1, 3), norm_g)
    z = z.reshape(Bn, L, H, DV).astype(jnp.float32)
    return (o * jax.nn.silu(z)).reshape(Bn, L, H * DV).astype(z.dtype if False else o.dtype)


def gdn_mixer(lat, ctx, conv_w, a_log, dt_bias, norm_g, ctx_out):
    ql, kl, vl, bl, gl = gdn_prep(lat, conv_w, a_log, dt_bias)
    qc, kc, vc, bc, gc = gdn_prep(ctx, conv_w, a_log, dt_bias)
    flip = lambda t: jnp.flip(t, axis=2)
    s0 = jnp.zeros((ql.shape[0], A_HEADS, A_DK, A_DV), jnp.float32)
    oc_f, sc_f = gated_delta_chunked(qc, kc, vc, bc[0], gc[0], s0)
    oc_b, sc_b = gated_delta_chunked(flip(qc), flip(kc), flip(vc), flip(bc[1]), flip(gc[1]), s0)
    ol_f, _ = gated_delta_chunked(ql, kl, vl, bl[0], gl[0], sc_f)
    ol_b, _ = gated_delta_chunked(flip(ql), flip(kl), flip(vl), flip(bl[1]), flip(gl[1]), sc_b)
    y_lat = gdn_out(ol_f + flip(ol_b), lat[3], norm_g).astype(lat[3].dtype)
    y_ctx = gdn_out(oc_f + flip(oc_b), ctx[3], norm_g).astype(ctx[3].dtype) if ctx_out else None
    return y_lat, y_ctx


def diff_attend(q, k, v, lam):
    s = jnp.einsum('bhmqd,bhmkd->bhmqk', q, k).astype(jnp.float32) * (B_DH ** -0.5)
    p = jax.nn.softmax(s, axis=-1)
    attn = p[:, :, 0] - lam * p[:, :, 1]
    return jnp.einsum('bhqk,bhkd->bhqd', attn.astype(v.dtype), v)


def diff_mixer(lat, ctx, lam_p, subln_g, lam_init, row_pos, col_pos, ctx_out):
    def heads_qk(t):
        return t.reshape(t.shape[0], t.shape[1], B_HEADS, 2, B_DH).transpose(0, 2, 3, 1, 4)

    def heads_v(t):
        return t.reshape(t.shape[0], t.shape[1], B_HEADS, 2 * B_DH).transpose(0, 2, 1, 3)

    ql = rope_2d(heads_qk(lat[0]), row_pos, col_pos)
    kl = rope_2d(heads_qk(lat[1]), row_pos, col_pos)
    vl = heads_v(lat[2])
    qc, kc, vc = heads_qk(ctx[0]), heads_qk(ctx[1]), heads_v(ctx[2])
    lp = lam_p.astype(jnp.float32)
    lam = jnp.exp(jnp.sum(lp[0] * lp[1])) - jnp.exp(jnp.sum(lp[2] * lp[3])) + lam_init
    k_all = jnp.concatenate([kl, kc], axis=3)
    v_all = jnp.concatenate([vl, vc], axis=2)
    Bn, H, _, L, Dh = ql.shape
    nb = L // Q_BLOCK
    qb = jnp.moveaxis(ql.reshape(Bn, H, 2, nb, Q_BLOCK, Dh), 3, 0)
    ol = lax.map(lambda qblk: diff_attend(qblk, k_all, v_all, lam), qb)
    ol = jnp.moveaxis(ol, 0, 2).reshape(Bn, H, L, 2 * Dh)

    def out(o):
        o = rmsnorm(o, subln_g) * (1 - lam_init)
        return o.transpose(0, 2, 1, 3).reshape(o.shape[0], o.shape[2], B_HEADS * 2 * B_DH)

    y_ctx = out(diff_attend(qc, kc, vc, lam)) if ctx_out else None
    return out(ol), y_ctx


def dense_attention(q, k, v):
    s = jnp.einsum('bhqd,bhkd->bhqk', q, k).astype(jnp.float32) * (q.shape[-1] ** -0.5)
    return jnp.einsum('bhqk,bhkd->bhqd', jax.nn.softmax(s, axis=-1).astype(v.dtype), v)


def neighbourhood_attention(q, k, v, kc, vc, rpb):
    Bn, H, L, Dh = q.shape
    rows = L // GRID_W
    wr = min(WIN_R, rows)
    grid = lambda t: t.reshape(Bn, H, rows, GRID_W, Dh)
    qg, kg, vg = grid(q), grid(k), grid(v)
    col = jnp.arange(GRID_W)
    col_start = jnp.clip(col - WIN_C // 2, 0, GRID_W - WIN_C)
    col_mask = (col[None, :] >= col_start[:, None]) & (col[None, :] < col_start[:, None] + WIN_C)
    mask = jnp.broadcast_to(col_mask[:, None, :], (GRID_W, wr, GRID_W)).reshape(GRID_W, wr * GRID_W)
    dc = jnp.clip(col[None, :] - col[:, None], -(WIN_C - 1), WIN_C - 1) + WIN_C - 1
    rpb = rpb.astype(jnp.float32)
    scale = Dh ** -0.5

    def row_block(r):
        rs = jnp.clip(r - WIN_R // 2, 0, rows - wr)
        kr = lax.dynamic_slice_in_dim(kg, rs, wr, axis=2).reshape(Bn, H, wr * GRID_W, Dh)
        vr = lax.dynamic_slice_in_dim(vg, rs, wr, axis=2).reshape(Bn, H, wr * GRID_W, Dh)
        qr = lax.dynamic_index_in_dim(qg, r, axis=2, keepdims=False)
        dr = rs + jnp.arange(wr) - r + WIN_R - 1
        bias = rpb[:, dr[:, None, None], dc[None, :, :]]
        bias = bias.transpose(0, 2, 1, 3).reshape(H, GRID_W, wr * GRID_W)
        s_lat = jnp.einsum('bhqd,bhkd->bhqk', qr, kr).astype(jnp.float32) * scale + bias
        s_lat = jnp.where(mask, s_lat, NEG_INF)
        s_ctx = jnp.einsum('bhqd,bhkd->bhqk', qr, kc).astype(jnp.float32) * scale
        p = jax.nn.softmax(jnp.concatenate([s_lat, s_ctx], axis=-1), axis=-1).astype(v.dtype)
        return jnp.einsum('bhqk,bhkd->bhqd', p, jnp.concatenate([vr, vc], axis=2))

    out = lax.map(row_block, jnp.arange(rows))
    return jnp.moveaxis(out, 0, 2).reshape(Bn, H, L, Dh)


def natten_mixer(lat, ctx, rpb, ctx_out):
    heads = lambda t: t.reshape(t.shape[0], t.shape[1], C_HEADS, C_DH).transpose(0, 2, 1, 3)
    flat = lambda o: o.transpose(0, 2, 1, 3).reshape(o.shape[0], o.shape[2], C_WIDTH)
    ql, kl, vl = heads(lat[0]), heads(lat[1]), heads(lat[2])
    qc, kc, vc = heads(ctx[0]), heads(ctx[1]), heads(ctx[2])
    y_lat = flat(neighbourhood_attention(ql, kl, vl, kc, vc, rpb))
    y_ctx = flat(dense_attention(qc, kc, vc)) if ctx_out else None
    return y_lat, y_ctx


def merge_branches(ya, yb, yc, gate_logits, w_up, w_out):
    y = jnp.stack([ya, yb, yc], axis=-2)
    up = jnp.einsum('blnw,nwd->blnd', y, w_up)
    gates = jax.nn.sigmoid(gate_logits.reshape(*gate_logits.shape[:-1], N_BRANCH, D_MODEL))
    return jnp.sum(gates * up, axis=-2) @ w_out


def peer_ffn(h, wq, keys, u, v):
    Bn, L, D = h.shape
    tok = h.reshape(Bn * L, D)
    T = Bn * L
    q = (tok @ wq).reshape(T, P_HEADS, 2, P_DKH)
    s = jnp.einsum('thpd,hpnd->thpn', q, keys).astype(jnp.float32)
    sv, si = lax.top_k(s, P_TOPK)
    cand_s = (sv[:, :, 0, :, None] + sv[:, :, 1, None, :]).reshape(T, P_HEADS, P_TOPK * P_TOPK)
    cand_i = (si[:, :, 0, :, None] * N_KEYS + si[:, :, 1, None, :]).reshape(T, P_HEADS, P_TOPK * P_TOPK)
    top_s, top_pos = lax.top_k(cand_s, P_TOPK)
    idx = jnp.take_along_axis(cand_i, top_pos, axis=-1)
    gate = jax.nn.softmax(top_s, axis=-1)
    nb = T // P_BLOCK

    def block(args):
        xb, ib, gb = args
        act = jax.nn.gelu(jnp.einsum('td,thkd->thk', xb, u[ib]).astype(jnp.float32), approximate=False)
        return jnp.einsum('thk,thkd->td', (gb * act).astype(v.dtype), v[ib])

    out = lax.map(block, (tok.reshape(nb, P_BLOCK, D), idx.reshape(nb, P_BLOCK, P_HEADS, P_TOPK),
                          gate.reshape(nb, P_BLOCK, P_HEADS, P_TOPK)))
    return out.reshape(Bn, L, D).astype(h.dtype)


def trunk_layer(x, xc, mod_lat, mod_ctx, n1_g, n2_g, w_in, conv_w, a_log, dt_bias, gdn_g,
                lam_p, subln_g, rpb, w_up, w_out, p_wq, p_keys, p_u, p_v, lam_init, ctx_out):
    L = x.shape[1]
    t = jnp.arange(L)
    row_pos, col_pos = t // GRID_W, t % GRID_W
    sh1, sc1, gt1, sh2, sc2, gt2 = jnp.split(mod_lat, 6, axis=-1)
    csh1, csc1, cgt1, csh2, csc2, cgt2 = jnp.split(mod_ctx, 6, axis=-1)
    pl = split_cols(modulate(rmsnorm(x, n1_g), sh1, sc1) @ w_in)
    pc = split_cols(modulate(rmsnorm(xc, n1_g), csh1, csc1) @ w_in)
    ya, ya_c = gdn_mixer(pl[0:6], pc[0:6], conv_w, a_log, dt_bias, gdn_g, ctx_out)
    yb, yb_c = diff_mixer(pl[6:9], pc[6:9], lam_p, subln_g, lam_init, row_pos, col_pos, ctx_out)
    yc, yc_c = natten_mixer(pl[9:12], pc[9:12], rpb, ctx_out)
    x = x + gt1 * merge_branches(ya, yb, yc, pl[12], w_up, w_out)
    x = x + gt2 * peer_ffn(modulate(rmsnorm(x, n2_g), sh2, sc2), p_wq, p_keys, p_u, p_v)
    if ctx_out:
        xc = xc + cgt1 * merge_branches(ya_c, yb_c, yc_c, pc[12], w_up, w_out)
        xc = xc + cgt2 * peer_ffn(modulate(rmsnorm(xc, n2_g), csh2, csc2), p_wq, p_keys, p_u, p_v)
    return x, xc


def setup_inputs(seed: int = 0) -> dict:
    key = jax.random.key(seed)
    ks = jax.random.split(key, 24)
    f32 = jnp.float32
    nrm = lambda k, shape, s: jax.random.normal(k, shape, f32) * s
    dt = jnp.exp(jax.random.uniform(ks[11], (DEPTH, 2, A_HEADS), f32, math.log(1e-3), math.log(1e-1)))
    return {
        'x': nrm(ks[0], (BATCH, SEQ, D_MODEL), 1.0),
        'c': nrm(ks[1], (BATCH, D_MODEL), 1.0),
        'ctx': nrm(ks[2], (BATCH, CTX_LEN, D_MODEL), 1.0),
        'c_ctx': nrm(ks[3], (D_MODEL,), 1.0),
        'norm1_g': 1.0 + nrm(ks[4], (DEPTH, D_MODEL), 0.01),
        'norm2_g': 1.0 + nrm(ks[5], (DEPTH, D_MODEL), 0.01),
        'ada_w': nrm(ks[6], (DEPTH, D_MODEL, 6 * D_MODEL), 0.5 * D_MODEL ** -0.5),
        'ada_b': nrm(ks[7], (DEPTH, 6 * D_MODEL), 0.01),
        'w_in': nrm(ks[8], (DEPTH, D_MODEL, IN_COLS), D_MODEL ** -0.5),
        'gdn_conv': nrm(ks[9], (DEPTH, CONV_K, 3 * A_WIDTH), CONV_K ** -0.5),
        'gdn_a_log': jnp.log(jax.random.uniform(ks[10], (DEPTH, 2, A_HEADS), f32, 1.0, 16.0)),
        'gdn_dt_bias': dt + jnp.log(-jnp.expm1(-dt)),
        'gdn_norm_g': 1.0 + nrm(ks[12], (DEPTH, A_DV), 0.01),
        'diff_lambda': nrm(ks[13], (DEPTH, 4, B_DH), 0.1),
        'diff_subln_g': 1.0 + nrm(ks[14], (DEPTH, 2 * B_DH), 0.01),
        'na_rpb': nrm(ks[15], (DEPTH, C_HEADS, 2 * WIN_R - 1, 2 * WIN_C - 1), 0.02),
        'w_up': nrm(ks[16], (DEPTH, N_BRANCH, BRANCH_W, D_MODEL), BRANCH_W ** -0.5),
        'w_out': nrm(ks[17], (DEPTH, D_MODEL, D_MODEL), D_MODEL ** -0.5),
        'peer_wq': nrm(ks[18], (DEPTH, D_MODEL, P_HEADS * 2 * P_DKH), D_MODEL ** -0.5),
        'peer_keys': nrm(ks[19], (DEPTH, P_HEADS, 2, N_KEYS, P_DKH), P_DKH ** -0.5),
        'peer_u': nrm(ks[20], (DEPTH, N_EXPERTS, D_MODEL), D_MODEL ** -0.5),
        'peer_v': nrm(ks[21], (DEPTH, N_EXPERTS, D_MODEL), P_HEADS ** -0.5),
        'final_g': 1.0 + nrm(ks[22], (D_MODEL,), 0.01),
    }


def reference(x, c, ctx, c_ctx, norm1_g, norm2_g, ada_w, ada_b, w_in, gdn_conv, gdn_a_log, gdn_dt_bias,
              gdn_norm_g, diff_lambda, diff_subln_g, na_rpb, w_up, w_out, peer_wq, peer_keys, peer_u, peer_v,
              final_g):
    xc = ctx
    for i in range(DEPTH):
        mod_lat = (jax.nn.silu(c) @ ada_w[i] + ada_b[i])[:, None, :]
        mod_ctx = jax.nn.silu(c_ctx) @ ada_w[i] + ada_b[i]
        lam_init = 0.8 - 0.6 * math.exp(-0.3 * i)
        x, xc = trunk_layer(x, xc, mod_lat, mod_ctx, norm1_g[i], norm2_g[i], w_in[i], gdn_conv[i],
                            gdn_a_log[i], gdn_dt_bias[i], gdn_norm_g[i], diff_lambda[i], diff_subln_g[i],
                            na_rpb[i], w_up[i], w_out[i], peer_wq[i], peer_keys[i], peer_u[i], peer_v[i],
                            lam_init, i < DEPTH - 1)
    return rmsnorm(x, final_g)
```

```python
import math
import numpy as np
from contextlib import ExitStack
import concourse.bass as bass
import concourse.mybir as mybir
from concourse.bass_utils import run_bass_kernel_spmd

F32 = mybir.dt.float32; BF16 = mybir.dt.bfloat16; I32 = mybir.dt.int32; U32 = mybir.dt.uint32
AF = mybir.ActivationFunctionType; ALU = mybir.AluOpType; AX = mybir.AxisListType

D = 1024; L = 4096; LC = 256; T = L + LC; NT = T // 128; DEPTH = 2
NP = 9232
O_AQ, O_AK, O_AV, O_AZ, O_BQ, O_BK, O_BV, O_CQ, O_CK, O_CV, O_G, O_BQP, O_BKP, O_BA = (
    0, 512, 1024, 1536, 2048, 2560, 3072, 3584, 4096, 4608, 5120, 8192, 8704, 9216)
EPS = 1e-6
SAME_ENGINE_SYNC = True
SKIP = set()
SEM_LIMIT = 6000
CUT = None
NAT_ROWS = 8


class Buf:
    __slots__ = ('name', 'w', 'r')

    def __init__(self, name):
        self.name = name; self.w = None; self.r = []


class DSem:
    __slots__ = ('sem', 'cnt', 'sw')

    def __init__(self, sem):
        self.sem = sem; self.cnt = 0; self.sw = False


class KB:
    def __init__(self, nc, es):
        self.nc = nc; self.es = es
        self.eng = {'pe': nc.tensor, 'act': nc.scalar, 'dve': nc.vector, 'pool': nc.gpsimd, 'sp': nc.sync}
        self.esem = {e: es.enter_context(nc.semaphore('se_' + e)) for e in ('pe', 'act', 'dve', 'pool')}
        self.ecnt = {e: 0 for e in self.esem}
        self.seen = {e: {} for e in self.eng}
        self.free_dsems = []
        self.free_dsems_sw = []
        self.all_dsems = []
        self.phase_sem = es.enter_context(nc.semaphore('phase'))
        self.phase_no = 0
        self.nbuf = 0
        self.ninst = 0
        self.all_bufs = []
        self.nrot = 0
        self.retired = []

    def buf(self, name=None):
        self.nbuf += 1
        b = Buf(name or f'b{self.nbuf}')
        self.all_bufs.append(b)
        return b

    def dsem(self, sw=False):
        if sw:
            if self.free_dsems_sw:
                return self.free_dsems_sw.pop()
        elif self.free_dsems:
            return self.free_dsems.pop()
        d = DSem(self.es.enter_context(self.nc.semaphore(f'ds{len(self.all_dsems)}')))
        self.all_dsems.append(d)
        return d

    def _wait(self, e, tok):
        sem, val = tok
        key = sem.num
        if self.seen[e].get(key, 0) >= val:
            return
        self.eng[e].wait_ge(sem, val)
        self.seen[e][key] = val

    def _deps(self, e, reads, writes):
        own = self.esem.get(e)
        for b in reads:
            if b.w is not None:
                if SAME_ENGINE_SYNC or b.w[0] is not own:
                    self._wait(e, b.w)
        for b in writes:
            if b.w is not None:
                if SAME_ENGINE_SYNC or b.w[0] is not own:
                    self._wait(e, b.w)
            for t in b.r:
                if SAME_ENGINE_SYNC or t[0] is not own:
                    self._wait(e, t)

    def _fresh_sem(self):
        self.nrot += 1
        return self.es.enter_context(self.nc.semaphore(f'rot{self.nrot}'))

    def op(self, e, reads, writes, fn):
        if self.ecnt[e] >= SEM_LIMIT:
            self.esem[e] = self._fresh_sem(); self.ecnt[e] = 0
        self._deps(e, reads, writes)
        ins = fn(self.eng[e])
        self.ecnt[e] += 1
        self.ninst += 1
        ins.then_inc(self.esem[e], 1)
        tok = (self.esem[e], self.ecnt[e])
        for b in reads:
            b.r.append(tok)
        for b in writes:
            b.w = tok; b.r = []
        return ins

    def dma(self, e, ds, reads, writes, fn):
        if ds.cnt >= SEM_LIMIT:
            self.retired.append((ds.sem, ds.cnt))
            ds.sem = self._fresh_sem(); ds.cnt = 0
        self._deps(e, reads, writes)
        ins = fn(self.eng[e])
        self.ninst += 1
        ds.cnt += 16
        ins.then_inc(ds.sem, 16)
        tok = (ds.sem, ds.cnt)
        for b in reads:
            b.r.append(tok)
        for b in writes:
            b.w = tok; b.r = []
        return ins

    def barrier(self, release=()):
        for e in self.esem:
            if self.ecnt[e] > 0:
                self._wait('sp', (self.esem[e], self.ecnt[e]))
        for d in self.all_dsems:
            if d.cnt > 0:
                self._wait('sp', (d.sem, d.cnt))
        for tok in self.retired:
            self._wait('sp', tok)
        self.retired = []
        self.phase_no += 1
        self.nc.sync.sem_inc(self.phase_sem, 1)
        for e in ('pe', 'act', 'dve', 'pool'):
            self.eng[e].wait_ge(self.phase_sem, self.phase_no)
        for d in release:
            (self.free_dsems_sw if getattr(d, 'sw', False) else self.free_dsems).append(d)


class Tile:
    def __init__(self, kb, t, ds=None):
        self.t = t; self.b = kb.buf(); self.ds = ds


class Phase:
    def __init__(self, kb):
        self.kb = kb; self.nc = kb.nc; self.es = ExitStack(); self.dsems = []; self.n = 0

    def __enter__(self):
        self.es.__enter__(); return self

    def __exit__(self, *a):
        self.kb.barrier(release=self.dsems)
        return self.es.__exit__(*a)

    def sb(self, shape, dt, dma=False, sw=False):
        self.n += 1
        t = self.es.enter_context(self.nc.sbuf_tensor(f'p{self.kb.phase_no}_s{self.n}', list(shape), dt))
        ds = None
        if dma:
            ds = self.kb.dsem(sw=sw); ds.sw = sw; self.dsems.append(ds)
        return Tile(self.kb, t, ds)

    def ps(self, shape, dt=F32):
        self.n += 1
        t = self.es.enter_context(self.nc.psum_tensor(f'p{self.kb.phase_no}_q{self.n}', list(shape), dt))
        return Tile(self.kb, t)

    def ring(self, n, shape, dt, dma=False, psum=False):
        return Ring([self.ps(shape, dt) if psum else self.sb(shape, dt, dma) for _ in range(n)])


class Ring:
    def __init__(self, tiles):
        self.tiles = tiles; self.i = 0

    def next(self):
        t = self.tiles[self.i % len(self.tiles)]; self.i += 1
        return t


def load(kb, tl, dst_ap, src_ap, eng='sp', slow=False):
    return kb.dma(eng, tl.ds, [], [tl.b], lambda e: e.dma_start(out=dst_ap, in_=src_ap, allow_slow_non_contiguous=slow))


def store(kb, tl, dst_ap, src_ap, eng='sp'):
    return kb.dma(eng, tl.ds, [tl.b], [], lambda e: e.dma_start(out=dst_ap, in_=src_ap))


def tok_rows(dr, t):
    return dr[t * 128:(t + 1) * 128]


def phase_mod(kb, dr, l):
    with Phase(kb) as ph:
        cc = ph.sb([128, 8, 2], F32, dma=True)
        cb = ph.sb([128, 8, 2], BF16)
        ab = ph.sb([2, 6144], F32, dma=True)
        mo = ph.sb([2, 6144], F32, dma=True)
        wr = ph.ring(2, [128, 8, 512], F32, dma=True)
        wbr = ph.ring(2, [128, 8, 512], BF16)
        pr = ph.ring(2, [2, 512], F32, psum=True)
        load(kb, cc, cc.t[:, :, 0], dr['c'].rearrange("(k p) -> p k", p=128), slow=True)
        load(kb, cc, cc.t[:, :, 1], dr['c_ctx'].rearrange("(k p) -> p k", p=128), slow=True)
        load(kb, ab, ab.t[0:1, :], dr['ada_b'][l:l + 1, :])
        load(kb, ab, ab.t[1:2, :], dr['ada_b'][l:l + 1, :])
        kb.op('act', [cc.b], [cb.b], lambda e: e.activation(out=cb.t[:], in_=cc.t[:], func=AF.Silu))
        for nb in range(12):
            w = wr.next(); wb = wbr.next(); p = pr.next()
            load(kb, w, w.t[:], dr['ada_w'][l, :, nb * 512:(nb + 1) * 512].rearrange("(k p) n -> p k n", p=128))
            kb.op('pool', [w.b], [wb.b], lambda e: e.tensor_copy(out=wb.t[:], in_=w.t[:]))
            for k in range(8):
                kb.op('pe', [cb.b, wb.b], [p.b], lambda e: e.matmul(p.t[:], lhsT=cb.t[:, k, :], rhs=wb.t[:, k, :], start=(k == 0), stop=(k == 7)))
            kb.op('dve', [p.b, ab.b], [mo.b], lambda e: e.tensor_tensor(out=mo.t[:, nb * 512:(nb + 1) * 512], in0=p.t[:], in1=ab.t[:, nb * 512:(nb + 1) * 512], op=ALU.add))
        store(kb, mo, dr['mod'][l], mo.t[:])


def bcast_rows(ap_row, n=128):
    return ap_row.partition_broadcast(n)


def rms_mod_tile(kb, ph, xt, A, Bm, out_tile, out_ap, scratch):
    junk, ss = scratch
    kb.op('act', [xt.b], [junk.b, ss.b], lambda e: e.activation(out=junk.t[:], in_=xt.t[:], func=AF.Square, accum_out=ss.t[:, 0:1]))
    kb.op('dve', [ss.b], [ss.b], lambda e: e.tensor_scalar(out=ss.t[:, 1:2], in0=ss.t[:, 0:1], scalar1=1.0 / D, scalar2=EPS, op0=ALU.mult, op1=ALU.add))
    kb.op('act', [ss.b], [ss.b], lambda e: e.sqrt(out=ss.t[:, 2:3], in_=ss.t[:, 1:2]))
    kb.op('dve', [ss.b], [ss.b], lambda e: e.reciprocal(out=ss.t[:, 3:4], in_=ss.t[:, 2:3]))
    kb.op('dve', [xt.b, ss.b, A.b], [junk.b], lambda e: e.scalar_tensor_tensor(out=junk.t[:], in0=xt.t[:], scalar=ss.t[:, 3:4], in1=A.t[:], op0=ALU.mult, op1=ALU.mult))
    kb.op('dve', [junk.b, Bm.b], [out_tile.b], lambda e: e.tensor_tensor(out=out_ap, in0=junk.t[:], in1=Bm.t[:], op=ALU.add))


def load_modAB(kb, ph, dr, l, which, g_name):
    res = []
    g = ph.sb([128, D], F32, dma=True)
    load(kb, g, g.t[:], bcast_rows(dr[g_name][l:l + 1, :]))
    for r in range(2):
        A = ph.sb([128, D], F32, dma=True); Bm = ph.sb([128, D], F32, dma=True)
        o = which * 3 * D
        load(kb, Bm, Bm.t[:], bcast_rows(dr['mod'][l, r:r + 1, o:o + D]))
        load(kb, A, A.t[:], bcast_rows(dr['mod'][l, r:r + 1, o + D:o + 2 * D]))
        kb.op('dve', [A.b, g.b], [A.b], lambda e: e.scalar_tensor_tensor(out=A.t[:], in0=A.t[:], scalar=1.0, in1=g.t[:], op0=ALU.add, op1=ALU.mult))
        res.append((A, Bm))
    return res


def x_src(dr, l, t):
    if l == 0:
        return dr['x'][t * 128:(t + 1) * 128] if t < 32 else dr['ctx'][(t - 32) * 128:(t - 31) * 128]
    return dr['xres'][t * 128:(t + 1) * 128]


def phase_inproj(kb, dr, l, ident):
    with Phase(kb) as ph:
        hT = ph.sb([128, 8, T], BF16)
        AB = load_modAB(kb, ph, dr, l, 0, 'norm1_g')
        xr = ph.ring(2, [128, D], F32, dma=True)
        junk = ph.sb([128, D], F32); ss = ph.sb([128, 4], F32)
        hbr = ph.ring(2, [128, D], BF16)
        ptr = ph.ring(2, [128, 8, 128], BF16, psum=True)
        for t in range(NT):
            xt = xr.next(); hb = hbr.next(); pt = ptr.next()
            load(kb, xt, xt.t[:], x_src(dr, l, t))
            A, Bm = AB[0] if t < 32 else AB[1]
            rms_mod_tile(kb, ph, xt, A, Bm, hb, hb.t[:], (junk, ss))
            for k in range(8):
                kb.op('pe', [hb.b, ident.b], [pt.b], lambda e: e.transpose(out=pt.t[:, k, :], in_=hb.t[:, k * 128:(k + 1) * 128], identity=ident.t[:]))
            kb.op('act', [pt.b], [hT.b], lambda e: e.copy(out=hT.t[:, :, t * 128:(t + 1) * 128], in_=pt.t[:]))
        wr = ph.ring(2, [128, 8, 512], F32, dma=True)
        wbr = ph.ring(2, [128, 8, 512], BF16)
        pr = ph.ring(4, [128, 512], F32, psum=True)
        orr = ph.ring(4, [128, 512], F32, dma=True)
        nblk = (NP + 511) // 512
        i = 0
        for cbk in range(nblk):
            c0 = cbk * 512; nc_ = min(512, NP - c0)
            w = wr.next(); wb = wbr.next()
            load(kb, w, w.t[:, :, :nc_], dr['w_ext'][l, :, c0:c0 + nc_].rearrange("(k p) n -> p k n", p=128))
            kb.op('pool', [w.b], [wb.b], lambda e: e.tensor_copy(out=wb.t[:, :, :nc_], in_=w.t[:, :, :nc_]))
            for t in range(NT):
                p = pr.next(); o = orr.next()
                for k in range(8):
                    kb.op('pe', [hT.b, wb.b], [p.b], lambda e: e.matmul(p.t[:, :nc_], lhsT=hT.t[:, k, t * 128:(t + 1) * 128], rhs=wb.t[:, k, :nc_], start=(k == 0), stop=(k == 7)))
                if i % 2 == 0:
                    kb.op('act', [p.b], [o.b], lambda e: e.copy(out=o.t[:, :nc_], in_=p.t[:, :nc_]))
                else:
                    kb.op('dve', [p.b], [o.b], lambda e: e.tensor_copy(out=o.t[:, :nc_], in_=p.t[:, :nc_]))
                i += 1
                store(kb, o, dr['proj'][t * 128:(t + 1) * 128, c0:c0 + nc_], o.t[:, :nc_])


def phase_diff(kb, dr, l, ident, ctx_out):
    lam_init = 0.8 - 0.6 * math.exp(-0.3 * l)
    with Phase(kb) as ph:
        rope = ph.sb([128, NT, 128], F32, dma=True)
        load(kb, rope, rope.t[:], dr['rope'].rearrange("(t p) c -> p t c", p=128))
        lp = ph.sb([128, 4, 64], F32, dma=True)
        load(kb, lp, lp.t[:], dr['diff_lambda'][l].partition_broadcast(128))
        lw = ph.sb([128, 2, 64], F32); ls = ph.sb([128, 4], F32)
        kb.op('dve', [lp.b], [lw.b], lambda e: e.tensor_tensor(out=lw.t[:, 0, :], in0=lp.t[:, 0, :], in1=lp.t[:, 1, :], op=ALU.mult))
        kb.op('dve', [lp.b], [lw.b], lambda e: e.tensor_tensor(out=lw.t[:, 1, :], in0=lp.t[:, 2, :], in1=lp.t[:, 3, :], op=ALU.mult))
        kb.op('dve', [lw.b], [ls.b], lambda e: e.reduce_sum(out=ls.t[:, 0:2], in_=lw.t[:], axis=AX.X))
        kb.op('act', [ls.b], [ls.b], lambda e: e.activation(out=ls.t[:, 2:4], in_=ls.t[:, 0:2], func=AF.Exp))
        kb.op('dve', [ls.b], [ls.b], lambda e: e.tensor_tensor(out=ls.t[:, 0:1], in0=ls.t[:, 3:4], in1=ls.t[:, 2:3], op=ALU.subtract))
        kb.op('dve', [ls.b], [ls.b], lambda e: e.tensor_scalar(out=ls.t[:, 1:2], in0=ls.t[:, 0:1], scalar1=-lam_init, scalar2=None, op0=ALU.add))
        gsub = ph.sb([128, 128], F32, dma=True)
        load(kb, gsub, gsub.t[:], dr['diff_subln_g'][l].partition_broadcast(128))
        kb.op('dve', [gsub.b], [gsub.b], lambda e: e.tensor_scalar(out=gsub.t[:], in0=gsub.t[:], scalar1=1.0 - lam_init, scalar2=None, op0=ALU.mult))
        qT = ph.sb([64, 2, T], BF16); kT = ph.sb([64, 2, T], BF16); vaug = ph.sb([128, NT, 130], BF16)
        kb.op('dve', [], [vaug.b], lambda e: e.memset(vaug.t[:, :, 128:130], 1.0))
        stg = ph.ring(2, [128, 5, 128], F32, dma=True)
        tmp = ph.ring(2, [128, 4, 128], F32)
        qk = ph.ring(2, [128, 2, 128], BF16)
        ptr = ph.ring(1, [64, 4, 128], BF16, psum=True)
        STr = ph.ring(2, [128, 512], F32, psum=True)
        Or = ph.ring(1, [128, 4, 512], F32, psum=True)
        PTr = ph.ring(3, [128, 512], BF16)
        om = ph.sb([128, 2, 4, 128], F32); rec = ph.sb([128, 8], F32)
        ob = ph.ring(2, [128, 4, 128], F32, dma=True); junk = ph.sb([128, 4, 128], F32)
        for h in range(1 if CUT else 4):
            for t in range(0 if CUT == 'diff_prep0' else NT):
                sg = stg.next(); tm = tmp.next(); qb_ = qk.next(); pt = ptr.next()
                rows = slice(t * 128, (t + 1) * 128)
                for j, off in enumerate((O_BQ, O_BQP, O_BK, O_BKP, O_BV)):
                    load(kb, sg, sg.t[:, j, :], dr['proj'][rows, off + h * 128: off + (h + 1) * 128])
                cosb = rope.t[:, t, 0:64].unsqueeze(1).to_broadcast([128, 2, 64])
                sinb = rope.t[:, t, 64:128].unsqueeze(1).to_broadcast([128, 2, 64])
                v4 = lambda ap: ap.rearrange("p (m d) -> p m d", m=2)
                for j in range(2):
                    kb.op('dve', [sg.b, rope.b], [tm.b], lambda e: e.tensor_tensor(out=v4(tm.t[:, 2 * j, :]), in0=v4(sg.t[:, 2 * j, :]), in1=cosb, op=ALU.mult))
                    kb.op('dve', [sg.b, rope.b], [tm.b], lambda e: e.tensor_tensor(out=v4(tm.t[:, 2 * j + 1, :]), in0=v4(sg.t[:, 2 * j + 1, :]), in1=sinb, op=ALU.mult))
                    kb.op('dve', [tm.b], [qb_.b], lambda e: e.tensor_tensor(out=qb_.t[:, j, :], in0=tm.t[:, 2 * j, :], in1=tm.t[:, 2 * j + 1, :], op=ALU.add))
                kb.op('act', [sg.b], [vaug.b], lambda e: e.copy(out=vaug.t[:, t, 0:128], in_=sg.t[:, 4, :]))
                if CUT == 'diff_prep1':
                    continue
                for j in range(2):
                    for m in range(2):
                        kb.op('pe', [qb_.b, ident.b], [pt.b], lambda e: e.transpose(out=pt.t[:, 2 * j + m, :], in_=qb_.t[:, j, m * 64:(m + 1) * 64], identity=ident.t[:]))
                kb.op('act', [pt.b], [qT.b], lambda e: e.copy(out=qT.t[:, :, rows], in_=pt.t[:, 0:2, :]))
                kb.op('act', [pt.b], [kT.b], lambda e: e.copy(out=kT.t[:, :, rows], in_=pt.t[:, 2:4, :]))
            blocks = [(qb0 * 512, 512, 0, NT) for qb0 in range(8)]
            if ctx_out:
                blocks.append((L, 256, 32, NT))
            if CUT and CUT.startswith('diff_prep'):
                blocks = []
            if CUT == 'diff_b1':
                blocks = blocks[:1]
            for (q0, nq, k0, k1) in blocks:
                ns = nq // 128
                for m in range(2):
                    O = Or.next()
                    for kt in range(k0, k1):
                        ST = STr.next(); PT = PTr.next()
                        kb.op('pe', [kT.b, qT.b], [ST.b], lambda e: e.matmul(ST.t[:, :nq], lhsT=kT.t[:, m, kt * 128:(kt + 1) * 128], rhs=qT.t[:, m, q0:q0 + nq], start=True, stop=True))
                        kb.op('act', [ST.b], [PT.b], lambda e: e.activation(out=PT.t[:, :nq], in_=ST.t[:, :nq], func=AF.Exp, scale=0.125))
                        for qs in range(ns):
                            kb.op('pe', [PT.b, vaug.b], [O.b], lambda e: e.matmul(O.t[:, qs, 0:129], lhsT=PT.t[:, qs * 128:(qs + 1) * 128], rhs=vaug.t[:, kt, 0:129], start=(kt == k0), stop=(kt == k1 - 1)))
                    kb.op('dve', [O.b], [rec.b], lambda e: e.reciprocal(out=rec.t[:, m * 4:m * 4 + ns], in_=O.t[:, 0:ns, 128]))
                    kb.op('dve', [O.b, rec.b], [om.b], lambda e: e.tensor_tensor(out=om.t[:, m, 0:ns, :], in0=O.t[:, 0:ns, 0:128], in1=rec.t[:, m * 4:m * 4 + ns].unsqueeze(2).to_broadcast([128, ns, 128]), op=ALU.mult))
                o = ob.next()
                kb.op('dve', [om.b, ls.b], [o.b], lambda e: e.scalar_tensor_tensor(out=o.t[:, 0:ns, :], in0=om.t[:, 1, 0:ns, :], scalar=ls.t[:, 1:2], in1=om.t[:, 0, 0:ns, :], op0=ALU.mult, op1=ALU.add))
                kb.op('dve', [o.b], [junk.b], lambda e: e.tensor_tensor(out=junk.t[:, 0:ns, :], in0=o.t[:, 0:ns, :], in1=o.t[:, 0:ns, :], op=ALU.mult))
                kb.op('dve', [junk.b], [rec.b], lambda e: e.reduce_sum(out=rec.t[:, 0:ns], in_=junk.t[:, 0:ns, :], axis=AX.X))
                kb.op('dve', [rec.b], [rec.b], lambda e: e.tensor_scalar(out=rec.t[:, 0:ns], in0=rec.t[:, 0:ns], scalar1=1.0 / 128, scalar2=EPS, op0=ALU.mult, op1=ALU.add))
                kb.op('act', [rec.b], [rec.b], lambda e: e.sqrt(out=rec.t[:, 0:ns], in_=rec.t[:, 0:ns]))
                kb.op('dve', [rec.b], [rec.b], lambda e: e.reciprocal(out=rec.t[:, 0:ns], in_=rec.t[:, 0:ns]))
                kb.op('dve', [o.b, rec.b], [o.b], lambda e: e.tensor_tensor(out=o.t[:, 0:ns, :], in0=o.t[:, 0:ns, :], in1=rec.t[:, 0:ns].unsqueeze(2).to_broadcast([128, ns, 128]), op=ALU.mult))
                kb.op('dve', [o.b, gsub.b], [o.b], lambda e: e.tensor_tensor(out=o.t[:, 0:ns, :], in0=o.t[:, 0:ns, :], in1=gsub.t[:].unsqueeze(1).to_broadcast([128, ns, 128]), op=ALU.mult))
                store(kb, o, dr['ybr'][q0:q0 + nq, 512 + h * 128: 512 + (h + 1) * 128].rearrange("(s p) c -> p s c", p=128), o.t[:, 0:ns, :], eng='sp')


def phase_natten(kb, dr, l, ident, ctx_out):
    with Phase(kb) as ph:
        mask = ph.sb([64, 64], F32, dma=True)
        load(kb, mask, mask.t[:], dr['na_mask'])
        bias = ph.sb([64, 15, 64], F32, dma=True)
        qT = ph.sb([64, T], BF16); kT = ph.sb([64, T], BF16); vr = ph.sb([64, 68, 66], BF16)
        kb.op('pool', [], [vr.b], lambda e: e.memset(vr.t[:, :, 64:66], 1.0))
        sq = ph.sb([128, NT, 64], F32, dma=True); sk = ph.sb([128, NT, 64], F32, dma=True); sv = ph.sb([64, 68, 64], F32, dma=True)
        sqb = ph.sb([128, NT, 64], BF16); skb = ph.sb([128, NT, 64], BF16)
        ptr = ph.ring(1, [64, 4, 128], BF16, psum=True)
        Sr = ph.ring(2, [64, 8, 64], F32, psum=True)
        Scr = ph.ring(1, [64, 4, 256], F32, psum=True)
        Or = ph.ring(2, [64, 7, 66], F32, psum=True)
        Sbr = ph.ring(2, [64, 8, 64], F32)
        Pr = ph.ring(2, [64, 8, 64], BF16); Pcr = ph.ring(2, [64, 4, 256], BF16)
        yo = ph.sb([64, 68, 64], F32, dma=True); rec = ph.sb([64, 8], F32)
        for h in range(1 if CUT else 8):
            load(kb, bias, bias.t[:], dr['rpbg'][l, h])
            kb.op('dve', [bias.b, mask.b], [bias.b], lambda e: e.tensor_tensor(out=bias.t[:], in0=bias.t[:], in1=mask.t[:].unsqueeze(1).to_broadcast([64, 15, 64]), op=ALU.add))
            load(kb, sq, sq.t[:], dr['proj'][:, O_CQ + h * 64:O_CQ + (h + 1) * 64].rearrange("(t p) c -> p t c", p=128))
            load(kb, sk, sk.t[:], dr['proj'][:, O_CK + h * 64:O_CK + (h + 1) * 64].rearrange("(t p) c -> p t c", p=128))
            load(kb, sv, sv.t[:], dr['proj'][:, O_CV + h * 64:O_CV + (h + 1) * 64].rearrange("(r p) c -> p r c", p=64))
            kb.op('dve', [sq.b], [sqb.b], lambda e: e.tensor_copy(out=sqb.t[:], in_=sq.t[:]))
            kb.op('pool', [sk.b], [skb.b], lambda e: e.tensor_copy(out=skb.t[:], in_=sk.t[:]))
            kb.op('act', [sv.b], [vr.b], lambda e: e.copy(out=vr.t[:, :, 0:64], in_=sv.t[:]))
            for (src, dst) in ((sqb, qT), (skb, kT)):
                for t0 in range(0, NT, 4):
                    n = min(4, NT - t0); pt = ptr.next()
                    for j in range(n):
                        kb.op('pe', [src.b, ident.b], [pt.b], lambda e: e.transpose(out=pt.t[:, j, :], in_=src.t[:, t0 + j, :], identity=ident.t[:]))
                    kb.op('act', [pt.b], [dst.b], lambda e: e.copy(out=dst.t[:, t0 * 128:(t0 + n) * 128].rearrange("p (j c) -> p j c", j=n), in_=pt.t[:, 0:n, :]))
            O = None
            for r in range(NAT_ROWS if CUT else 64):
                rs = min(max(r - 4, 0), 56); dr0 = rs - r + 7
                if r % 7 == 0:
                    O = Or.next(); rbase = r
                S = Sr.next(); Sc = Scr.next(); Sb = Sbr.next(); P = Pr.next(); Pc = Pcr.next()
                qs_ = qT.t[:, r * 64:(r + 1) * 64]
                for i in range(8):
                    kb.op('pe', [kT.b, qT.b], [S.b], lambda e: e.matmul(S.t[:, i, :], lhsT=kT.t[:, (rs + i) * 64:(rs + i + 1) * 64], rhs=qs_, start=True, stop=True))
                for i in range(4):
                    kb.op('pe', [kT.b, qT.b], [Sc.b], lambda e: e.matmul(Sc.t[:, i, 0:64], lhsT=kT.t[:, L + i * 64:L + (i + 1) * 64], rhs=qs_, start=True, stop=True))
                kb.op('dve', [S.b, bias.b], [Sb.b], lambda e: e.scalar_tensor_tensor(out=Sb.t[:], in0=S.t[:], scalar=0.125, in1=bias.t[:, dr0:dr0 + 8, :], op0=ALU.mult, op1=ALU.add))
                kb.op('act', [Sb.b], [P.b], lambda e: e.activation(out=P.t[:], in_=Sb.t[:], func=AF.Exp))
                kb.op('act', [Sc.b], [Pc.b], lambda e: e.activation(out=Pc.t[:, :, 0:64], in_=Sc.t[:, :, 0:64], func=AF.Exp, scale=0.125))
                oi = r - rbase
                for i in range(12):
                    lhs = P.t[:, i, :] if i < 8 else Pc.t[:, i - 8, 0:64]
                    vrow = rs + i if i < 8 else 64 + (i - 8)
                    kb.op('pe', [P.b, Pc.b, vr.b], [O.b], lambda e: e.matmul(O.t[:, oi, 0:65], lhsT=lhs, rhs=vr.t[:, vrow, 0:65], start=(i == 0), stop=(i == 11)))
                if oi == 6 or r == (NAT_ROWS if CUT else 64) - 1:
                    n = oi + 1
                    kb.op('dve', [O.b], [rec.b], lambda e: e.reciprocal(out=rec.t[:, 0:n], in_=O.t[:, 0:n, 64]))
                    kb.op('dve', [O.b, rec.b], [yo.b], lambda e: e.tensor_tensor(out=yo.t[:, rbase:rbase + n, :], in0=O.t[:, 0:n, 0:64], in1=rec.t[:, 0:n].unsqueeze(2).to_broadcast([64, n, 64]), op=ALU.mult))
            nrows = 64
            if ctx_out:
                nrows = 68
                Sc = Scr.next(); Pc = Pcr.next(); O = Or.next()
                for i in range(4):
                    kb.op('pe', [kT.b, qT.b], [Sc.b], lambda e: e.matmul(Sc.t[:, i, :], lhsT=kT.t[:, L + i * 64:L + (i + 1) * 64], rhs=qT.t[:, L:T], start=True, stop=True))
                kb.op('act', [Sc.b], [Pc.b], lambda e: e.activation(out=Pc.t[:], in_=Sc.t[:], func=AF.Exp, scale=0.125))
                for qs in range(4):
                    for i in range(4):
                        kb.op('pe', [Pc.b, vr.b], [O.b], lambda e: e.matmul(O.t[:, qs, 0:65], lhsT=Pc.t[:, i, qs * 64:(qs + 1) * 64], rhs=vr.t[:, 64 + i, 0:65], start=(i == 0), stop=(i == 3)))
                kb.op('dve', [O.b], [rec.b], lambda e: e.reciprocal(out=rec.t[:, 0:4], in_=O.t[:, 0:4, 64]))
                kb.op('dve', [O.b, rec.b], [yo.b], lambda e: e.tensor_tensor(out=yo.t[:, 64:68, :], in0=O.t[:, 0:4, 0:64], in1=rec.t[:, 0:4].unsqueeze(2).to_broadcast([64, 4, 64]), op=ALU.mult))
            store(kb, yo, dr['ybr'][0:nrows * 64, 1024 + h * 64:1024 + (h + 1) * 64].rearrange("(r p) c -> p r c", p=64), yo.t[:, 0:nrows, :], eng='sp')


def phase_gdn_prep(kb, dr, l):
    with Phase(kb) as ph:
        cw = ph.sb([64, 5, 1536], F32, dma=True)
        load(kb, cw, cw.t[:], dr['gdn_conv'][l].partition_broadcast(64))
        dtb = ph.sb([64, 8], F32, dma=True); nA = ph.sb([64, 8], F32, dma=True)
        load(kb, dtb, dtb.t[:], dr['gdn_dt_bias'][l].rearrange("a b -> (a b)").partition_broadcast(64))
        load(kb, nA, nA.t[:], dr['gdn_a_log'][l].rearrange("a b -> (a b)").partition_broadcast(64))
        kb.op('act', [nA.b], [nA.b], lambda e: e.activation(out=nA.t[:], in_=nA.t[:], func=AF.Exp))
        kb.op('dve', [nA.b], [nA.b], lambda e: e.tensor_scalar(out=nA.t[:], in0=nA.t[:], scalar1=-1.0, scalar2=None, op0=ALU.mult))
        ba = ph.sb([64, 68, 16], F32, dma=True); bg = ph.sb([64, 68, 16], F32, dma=True)
        load(kb, ba, ba.t[:], dr['proj'][:, O_BA:O_BA + 16].rearrange("(c p) k -> p c k", p=64))
        kb.op('act', [ba.b], [bg.b], lambda e: e.activation(out=bg.t[:, :, 0:8], in_=ba.t[:, :, 0:8], func=AF.Sigmoid))
        kb.op('dve', [ba.b, dtb.b], [ba.b], lambda e: e.tensor_tensor(out=ba.t[:, :, 8:16], in0=ba.t[:, :, 8:16], in1=dtb.t[:].unsqueeze(1).to_broadcast([64, 68, 8]), op=ALU.add))
        kb.op('act', [ba.b], [ba.b], lambda e: e.activation(out=ba.t[:, :, 8:16], in_=ba.t[:, :, 8:16], func=AF.Exp))
        kb.op('act', [ba.b], [ba.b], lambda e: e.activation(out=ba.t[:, :, 8:16], in_=ba.t[:, :, 8:16], func=AF.Ln, bias=1.0))
        kb.op('dve', [ba.b, nA.b], [bg.b], lambda e: e.tensor_tensor(out=bg.t[:, :, 8:16], in0=ba.t[:, :, 8:16], in1=nA.t[:].unsqueeze(1).to_broadcast([64, 68, 8]), op=ALU.mult))
        store(kb, bg, dr['gbg'].rearrange("(c p) k -> p c k", p=64), bg.t[:], eng='sp')
        xsr = ph.ring(2, [64, 5, 1536], F32, dma=True)
        acc = ph.sb([64, 1536], F32); tmpr = ph.ring(2, [64, 1536], F32); sact = ph.sb([64, 1536], F32)
        sq = ph.sb([64, 1024], F32); n8 = ph.sb([64, 8], F32)
        outr = ph.ring(2, [64, 1536], F32, dma=True)
        for c in range(68):
            seg_lo, seg_hi = (0, L) if c < 64 else (L, T)
            xs = xsr.next(); o = outr.next()
            for j in range(5):
                r0 = c * 64 + j - 2
                lo = max(r0, seg_lo); hi = min(r0 + 64, seg_hi)
                if lo > r0 or hi < r0 + 64:
                    kb.op('pool', [], [xs.b], lambda e: e.memset(xs.t[:, j, :], 0.0))
                load(kb, xs, xs.t[lo - r0:hi - r0, j, :], dr['proj'][lo:hi, 0:1536])
            kb.op('dve', [xs.b, cw.b], [acc.b], lambda e: e.tensor_tensor(out=acc.t[:], in0=xs.t[:, 0, :], in1=cw.t[:, 0, :], op=ALU.mult))
            for j in range(1, 5):
                tm = tmpr.next()
                kb.op('dve', [xs.b, cw.b], [tm.b], lambda e: e.tensor_tensor(out=tm.t[:], in0=xs.t[:, j, :], in1=cw.t[:, j, :], op=ALU.mult))
                kb.op('dve', [acc.b, tm.b], [acc.b], lambda e: e.tensor_tensor(out=acc.t[:], in0=acc.t[:], in1=tm.t[:], op=ALU.add))
            kb.op('act', [acc.b], [sact.b], lambda e: e.activation(out=sact.t[:], in_=acc.t[:], func=AF.Silu))
            kb.op('dve', [sact.b], [sq.b], lambda e: e.tensor_tensor(out=sq.t[:], in0=sact.t[:, 0:1024], in1=sact.t[:, 0:1024], op=ALU.mult))
            kb.op('dve', [sq.b], [n8.b], lambda e: e.reduce_sum(out=n8.t[:], in_=sq.t[:].rearrange("p (h d) -> p h d", h=8), axis=AX.X))
            kb.op('dve', [n8.b], [n8.b], lambda e: e.tensor_scalar(out=n8.t[:], in0=n8.t[:], scalar1=EPS, scalar2=None, op0=ALU.add))
            kb.op('act', [n8.b], [n8.b], lambda e: e.sqrt(out=n8.t[:], in_=n8.t[:]))
            kb.op('dve', [n8.b], [n8.b], lambda e: e.reciprocal(out=n8.t[:], in_=n8.t[:]))
            kb.op('dve', [n8.b], [n8.b], lambda e: e.tensor_scalar(out=n8.t[:, 0:4], in0=n8.t[:, 0:4], scalar1=128.0 ** -0.5, scalar2=None, op0=ALU.mult))
            kb.op('dve', [sact.b, n8.b], [o.b], lambda e: e.tensor_tensor(out=o.t[:, 0:1024].rearrange("p (h d) -> p h d", h=8), in0=sact.t[:, 0:1024].rearrange("p (h d) -> p h d", h=8),
                  in1=n8.t[:].unsqueeze(2).to_broadcast([64, 8, 128]), op=ALU.mult))
            kb.op('act', [sact.b], [o.b], lambda e: e.copy(out=o.t[:, 1024:1536], in_=sact.t[:, 1024:1536]))
            store(kb, o, dr['gqkv'][c * 64:(c + 1) * 64, :], o.t[:], eng='sp')


def phase_gdn(kb, dr, l, ident):
    NCH = 68
    with Phase(kb) as ph:
        id64 = ident.t[0:64, 0:64]
        gcn = ph.sb([64, 5, 64], F32, dma=True)
        load(kb, gcn, gcn.t[:], dr['gconst'])
        Lb16 = ph.sb([64, 2, 64], BF16); ones16 = ph.sb([64, 128], BF16)
        kb.op('dve', [gcn.b], [Lb16.b], lambda e: e.tensor_copy(out=Lb16.t[:], in_=gcn.t[:, 0:2, :]))
        kb.op('pool', [], [ones16.b], lambda e: e.memset(ones16.t[:], 1.0))
        gg = ph.sb([64, 128], F32, dma=True)
        load(kb, gg, gg.t[:], dr['gdn_norm_g'][l].partition_broadcast(64))
        bgall = ph.sb([64, NCH, 16], F32, dma=True)
        load(kb, bgall, bgall.t[:], dr['gbg'].rearrange("(c p) k -> p c k", p=64))
        banks = ph.ring(7, [128, 512], F32, psum=True)
        bt = ph.ps([128, 8, 64], BF16)
        v3 = lambda bank, np_, a, b: bank.t[0:np_, 0:a * b].rearrange("p (a b) -> p a b", b=b)
        ghi = ph.sb([64, NCH, 8], BF16); glo = ph.sb([64, NCH, 8], BF16); gtmp = ph.sb([64, NCH, 8], F32)
        gc = ph.sb([64, NCH, 8], F32); glB = ph.sb([64, NCH, 8], F32); Sdec = ph.sb([128, NCH, 8], F32)
        egc = ph.sb([64, NCH, 8], F32); kds = ph.sb([64, NCH, 8], F32); bgs = ph.sb([64, NCH, 8], F32); nbeta = ph.sb([64, NCH, 8], F32)
        gv = bgall.t[:, :, 8:16]
        kb.op('dve', [bgall.b], [ghi.b], lambda e: e.tensor_copy(out=ghi.t[:], in_=gv))
        kb.op('dve', [ghi.b], [gtmp.b], lambda e: e.tensor_copy(out=gtmp.t[:], in_=ghi.t[:]))
        kb.op('dve', [bgall.b, gtmp.b], [glo.b], lambda e: e.tensor_tensor(out=glo.t[:], in0=gv, in1=gtmp.t[:], op=ALU.subtract))
        for half in range(2):
            cs = slice(half * 34, (half + 1) * 34)
            outs = []
            for (lhs, np_) in ((Lb16.t[:, 0, :], 64), (Lb16.t[:, 1, :], 64), (ones16.t[:, 0:64], 64), (ones16.t[:], 128)):
                pb = banks.next(); outs.append(pb)
                kb.op('pe', [Lb16.b, ones16.b, ghi.b], [pb.b], lambda e: e.matmul(v3(pb, np_, 34, 8), lhsT=lhs, rhs=ghi.t[:, cs, :], start=True, stop=False))
                kb.op('pe', [Lb16.b, ones16.b, glo.b], [pb.b], lambda e: e.matmul(v3(pb, np_, 34, 8), lhsT=lhs, rhs=glo.t[:, cs, :], start=False, stop=True))
            kb.op('act', [outs[0].b], [gc.b], lambda e: e.copy(out=gc.t[:, cs, 0:4], in_=v3(outs[0], 64, 34, 8)[:, :, 0:4]))
            kb.op('act', [outs[1].b], [gc.b], lambda e: e.copy(out=gc.t[:, cs, 4:8], in_=v3(outs[1], 64, 34, 8)[:, :, 4:8]))
            kb.op('dve', [outs[2].b], [glB.b], lambda e: e.tensor_copy(out=glB.t[:, cs, :], in_=v3(outs[2], 64, 34, 8)))
            kb.op('act', [outs[3].b], [Sdec.b], lambda e: e.activation(out=Sdec.t[:, cs, :], in_=v3(outs[3], 128, 34, 8), func=AF.Exp))
        kb.op('act', [gc.b], [egc.b], lambda e: e.activation(out=egc.t[:], in_=gc.t[:], func=AF.Exp))
        kb.op('dve', [glB.b, gc.b], [kds.b], lambda e: e.tensor_tensor(out=kds.t[:], in0=glB.t[:], in1=gc.t[:], op=ALU.subtract))
        kb.op('act', [kds.b], [kds.b], lambda e: e.activation(out=kds.t[:], in_=kds.t[:], func=AF.Exp))
        kb.op('dve', [bgall.b, egc.b], [bgs.b], lambda e: e.tensor_tensor(out=bgs.t[:], in0=bgall.t[:, :, 0:8], in1=egc.t[:], op=ALU.mult))
        kb.op('dve', [bgall.b], [nbeta.b], lambda e: e.tensor_scalar(out=nbeta.t[:], in0=bgall.t[:, :, 0:8], scalar1=-1.0, scalar2=None, op0=ALU.mult))
        k_tok = ph.sb([64, NCH, 128], BF16); v_tok = ph.sb([64, NCH, 128], BF16)
        kT = ph.sb([128, NCH * 64], BF16); qT = ph.sb([128, NCH * 64], BF16)
        stg = ph.sb([64, 17, 128], F32, dma=True); qst = ph.sb([64, 17, 128], BF16)
        TT = ph.sb([64, NCH, 64], BF16); P2 = ph.sb([64, NCH, 64], BF16); negwT = ph.sb([128, NCH * 64], BF16)
        o_all = ph.sb([64, NCH, 128], F32, dma=True)
        gch = ph.sb([64, 8], BF16); gchf = ph.sb([64, 8], F32); gcl = ph.sb([64, 8], BF16)
        gcrh = ph.sb([64, 8, 64], BF16); gcrl = ph.sb([64, 8, 64], BF16)
        Xs = ph.sb([64, 8, 64], F32); Xp = ph.sb([64, 8, 64], F32); Xn = ph.sb([64, 8, 64], F32)
        xr = ph.ring(3, [64, 8, 64], BF16); yr_ = ph.ring(3, [64, 8, 64], BF16); rr = ph.ring(3, [64, 8, 64], BF16)
        kbg = ph.sb([64, 8, 128], BF16)
        S = ph.sb([128, 128], F32); Sb = ph.sb([128, 128], BF16)
        vbr = ph.ring(2, [64, 128], BF16); kdr = ph.ring(2, [64, 128], BF16); vnr = ph.ring(2, [64, 128], BF16)
        t1r = ph.ring(2, [64, 128], F32); t2r = ph.ring(2, [64, 128], F32)
        rs17 = ph.sb([64, 17], F32); sq17 = Xs
        print('gdn sbuf bytes remaining', kb.nc.sbuf_bytes_remaining)
        for h in range(1 if CUT else 4):
            for (col0, kind) in ((512, 'k'), (1024, 'v'), (0, 'q')):
                for pc in range(4):
                    c0 = pc * 17
                    load(kb, stg, stg.t[:], dr['gqkv'][c0 * 64:(c0 + 17) * 64, col0 + h * 128:col0 + (h + 1) * 128].rearrange("(c p) d -> p c d", p=64))
                    if kind == 'k':
                        kb.op('dve', [stg.b], [k_tok.b], lambda e: e.tensor_copy(out=k_tok.t[:, c0:c0 + 17, :], in_=stg.t[:]))
                    elif kind == 'v':
                        kb.op('dve', [stg.b], [v_tok.b], lambda e: e.tensor_copy(out=v_tok.t[:, c0:c0 + 17, :], in_=stg.t[:]))
                    else:
                        kb.op('dve', [stg.b], [qst.b], lambda e: e.tensor_copy(out=qst.t[:], in_=stg.t[:]))
                    if kind in ('k', 'q'):
                        src = k_tok if kind == 'k' else qst
                        dst = kT if kind == 'k' else qT
                        for g0 in range(0, 17, 8):
                            n = min(8, 17 - g0)
                            for j in range(n):
                                cc = (c0 + g0 + j) if kind == 'k' else (g0 + j)
                                kb.op('pe', [src.b, ident.b], [bt.b], lambda e: e.transpose(out=bt.t[:, j, :], in_=src.t[:, cc, :], identity=id64))
                            kb.op('act', [bt.b], [dst.b], lambda e: e.copy(out=dst.t[:, (c0 + g0) * 64:(c0 + g0 + n) * 64].rearrange("p (a b) -> p a b", b=64), in_=bt.t[:, 0:n, :]))
            for d in range(2):
                hd = d * 4 + h
                LmT = gcn.t[:, d, :]; Sm = gcn.t[:, 2 + d, :]
                for c0 in range(0, (16 if CUT == 'x' else NCH), 8):
                    n = min(8, NCH - c0)
                    csl = slice(c0, c0 + n)
                    bc = lambda ap2: ap2.unsqueeze(2).to_broadcast([64, n, 64])
                    bm = lambda ap2: ap2.unsqueeze(1).to_broadcast([64, n, 64])
                    kb.op('dve', [gc.b], [gch.b], lambda e: e.tensor_copy(out=gch.t[:, 0:n], in_=gc.t[:, csl, hd]))
                    kb.op('dve', [gch.b], [gchf.b], lambda e: e.tensor_copy(out=gchf.t[:, 0:n], in_=gch.t[:, 0:n]))
                    kb.op('dve', [gc.b, gchf.b], [gcl.b], lambda e: e.tensor_tensor(out=gcl.t[:, 0:n], in0=gc.t[:, csl, hd], in1=gchf.t[:, 0:n], op=ALU.subtract))
                    kb.op('dve', [gch.b], [gcrh.b], lambda e: e.tensor_copy(out=gcrh.t[:, 0:n, :], in_=bc(gch.t[:, 0:n])))
                    kb.op('dve', [gcl.b], [gcrl.b], lambda e: e.tensor_copy(out=gcrl.t[:, 0:n, :], in_=bc(gcl.t[:, 0:n])))
                    pG = banks.next(); pKK = banks.next(); pQK = banks.next()
                    for j in range(n):
                        c = c0 + j; cc = slice(c * 64, (c + 1) * 64)
                        kb.op('pe', [gcrh.b, ident.b], [pG.b], lambda e: e.matmul(v3(pG, 64, n, 64)[:, j, :], lhsT=gcrh.t[:, j, :], rhs=id64, start=True, stop=False))
                        kb.op('pe', [gcrl.b, ident.b], [pG.b], lambda e: e.matmul(v3(pG, 64, n, 64)[:, j, :], lhsT=gcrl.t[:, j, :], rhs=id64, start=False, stop=True))
                        kb.op('pe', [kT.b], [pKK.b], lambda e: e.matmul(v3(pKK, 64, n, 64)[:, j, :], lhsT=kT.t[:, cc], rhs=kT.t[:, cc], start=True, stop=True))
                        kb.op('pe', [kT.b, qT.b], [pQK.b], lambda e: e.matmul(v3(pQK, 64, n, 64)[:, j, :], lhsT=kT.t[:, cc], rhs=qT.t[:, cc], start=True, stop=True))
                    kb.op('dve', [pG.b, gc.b], [Xs.b], lambda e: e.tensor_tensor(out=Xs.t[:, 0:n, :], in0=v3(pG, 64, n, 64), in1=bc(gc.t[:, csl, hd]), op=ALU.subtract))
                    kb.op('dve', [Xs.b], [Xp.b], lambda e: e.tensor_scalar(out=Xp.t[:, 0:n, :], in0=Xs.t[:, 0:n, :], scalar1=0.0, scalar2=None, op0=ALU.max))
                    kb.op('dve', [Xs.b], [Xn.b], lambda e: e.tensor_scalar(out=Xn.t[:, 0:n, :], in0=Xs.t[:, 0:n, :], scalar1=0.0, scalar2=None, op0=ALU.min))
                    kb.op('act', [Xp.b], [Xp.b], lambda e: e.activation(out=Xp.t[:, 0:n, :], in_=Xp.t[:, 0:n, :], func=AF.Exp, scale=-1.0))
                    kb.op('act', [Xn.b], [Xn.b], lambda e: e.activation(out=Xn.t[:, 0:n, :], in_=Xn.t[:, 0:n, :], func=AF.Exp))
                    kb.op('dve', [Xp.b, gcn.b], [Xp.b], lambda e: e.tensor_tensor(out=Xp.t[:, 0:n, :], in0=Xp.t[:, 0:n, :], in1=bm(Sm), op=ALU.mult))
                    kb.op('dve', [Xp.b, nbeta.b], [Xp.b], lambda e: e.tensor_tensor(out=Xp.t[:, 0:n, :], in0=Xp.t[:, 0:n, :], in1=bc(nbeta.t[:, csl, hd]), op=ALU.mult))
                    X = xr.next()
                    kb.op('dve', [pKK.b, Xp.b], [X.b], lambda e: e.tensor_tensor(out=X.t[:, 0:n, :], in0=v3(pKK, 64, n, 64), in1=Xp.t[:, 0:n, :], op=ALU.mult))
                    kb.op('dve', [Xn.b, gcn.b], [Xn.b], lambda e: e.tensor_tensor(out=Xn.t[:, 0:n, :], in0=Xn.t[:, 0:n, :], in1=bm(LmT), op=ALU.mult))
                    kb.op('dve', [pQK.b, Xn.b], [P2.b], lambda e: e.tensor_tensor(out=P2.t[:, csl, :], in0=v3(pQK, 64, n, 64), in1=Xn.t[:, 0:n, :], op=ALU.mult))
                    for j in range(n):
                        kb.op('pe', [X.b, ident.b], [bt.b], lambda e: e.transpose(out=bt.t[0:64, j, :], in_=X.t[:, j, :], identity=id64))
                    Y = yr_.next(); R = rr.next()
                    kb.op('act', [bt.b], [Y.b], lambda e: e.copy(out=Y.t[:, 0:n, :], in_=bt.t[0:64, 0:n, :]))
                    kb.op('dve', [Y.b, ident.b], [R.b], lambda e: e.tensor_tensor(out=R.t[:, 0:n, :], in0=Y.t[:, 0:n, :], in1=bm(id64), op=ALU.add))
                    for m in range(5):
                        pX = banks.next(); Xn_ = xr.next()
                        for j in range(n):
                            kb.op('pe', [X.b, Y.b], [pX.b], lambda e: e.matmul(v3(pX, 64, n, 64)[:, j, :], lhsT=Y.t[:, j, :], rhs=X.t[:, j, :], start=True, stop=True))
                        kb.op('act', [pX.b], [Xn_.b], lambda e: e.copy(out=Xn_.t[:, 0:n, :], in_=v3(pX, 64, n, 64)))
                        if m < 4:
                            pY = banks.next(); Yn_ = yr_.next()
                            for j in range(n):
                                kb.op('pe', [X.b, Y.b], [pY.b], lambda e: e.matmul(v3(pY, 64, n, 64)[:, j, :], lhsT=X.t[:, j, :], rhs=Y.t[:, j, :], start=True, stop=True))
                            kb.op('dve', [pY.b], [Yn_.b], lambda e: e.tensor_copy(out=Yn_.t[:, 0:n, :], in_=v3(pY, 64, n, 64)))
                        pR = banks.next()
                        for j in range(n):
                            kb.op('pe', [R.b, ident.b], [pR.b], lambda e: e.matmul(v3(pR, 64, n, 64)[:, j, :], lhsT=id64, rhs=R.t[:, j, :], start=True, stop=False))
                            kb.op('pe', [R.b, Xn_.b], [pR.b], lambda e: e.matmul(v3(pR, 64, n, 64)[:, j, :], lhsT=Xn_.t[:, j, :], rhs=R.t[:, j, :], start=False, stop=True))
                        if m < 4:
                            Rn_ = rr.next()
                            kb.op('act', [pR.b], [Rn_.b], lambda e: e.copy(out=Rn_.t[:, 0:n, :], in_=v3(pR, 64, n, 64)))
                            R = Rn_; Y = Yn_
                        else:
                            kb.op('act', [pR.b], [TT.b], lambda e: e.copy(out=TT.t[:, csl, :], in_=v3(pR, 64, n, 64)))
                        X = Xn_
                    kb.op('dve', [k_tok.b, bgs.b], [kbg.b], lambda e: e.tensor_tensor(out=kbg.t[:, 0:n, :], in0=k_tok.t[:, csl, :], in1=bgs.t[:, csl, hd].unsqueeze(2).to_broadcast([64, n, 128]), op=ALU.mult))
                    pW = banks.next()
                    for j in range(n):
                        kb.op('pe', [kbg.b, TT.b], [pW.b], lambda e: e.matmul(v3(pW, 128, n, 64)[:, j, :], lhsT=kbg.t[:, j, :], rhs=TT.t[:, c0 + j, :], start=True, stop=True))
                    kb.op('act', [pW.b], [negwT.b], lambda e: e.mul(out=negwT.t[:, c0 * 64:(c0 + n) * 64].rearrange("p (a b) -> p a b", b=64), in_=v3(pW, 128, n, 64), mul=-1.0))
                kb.op('pool', [], [S.b], lambda e: e.memset(S.t[:], 0.0))
                kb.op('pool', [], [Sb.b], lambda e: e.memset(Sb.t[:], 0.0))
                order = ([64, 65, 66, 67] + list(range(64))) if d == 0 else ([67, 66, 65, 64] + list(range(63, -1, -1)))
                if CUT == 'x':
                    order = list(range(16)) if d == 0 else list(range(15, -1, -1))
                for c in order:
                    cc = slice(c * 64, (c + 1) * 64)
                    vb = vbr.next(); kd = kdr.next(); vn = vnr.next(); t1 = t1r.next()
                    kb.op('dve', [v_tok.b, bgall.b], [vb.b], lambda e: e.tensor_scalar(out=vb.t[:], in0=v_tok.t[:, c, :], scalar1=bgall.t[:, c, hd:hd + 1], scalar2=None, op0=ALU.mult))
                    kb.op('dve', [k_tok.b, kds.b], [kd.b], lambda e: e.tensor_scalar(out=kd.t[:], in0=k_tok.t[:, c, :], scalar1=kds.t[:, c, hd:hd + 1], scalar2=None, op0=ALU.mult))
                    pV = banks.next()
                    kb.op('pe', [TT.b, vb.b], [pV.b], lambda e: e.matmul(pV.t[0:64, 0:128], lhsT=TT.t[:, c, :], rhs=vb.t[:], start=True, stop=False))
                    kb.op('pe', [negwT.b, Sb.b], [pV.b], lambda e: e.matmul(pV.t[0:64, 0:128], lhsT=negwT.t[:, cc], rhs=Sb.t[:], start=False, stop=True))
                    kb.op('act', [pV.b], [vn.b], lambda e: e.copy(out=vn.t[:], in_=pV.t[0:64, 0:128]))
                    pO1 = banks.next(); pO2 = banks.next(); pS = banks.next()
                    kb.op('pe', [qT.b, Sb.b], [pO1.b], lambda e: e.matmul(pO1.t[0:64, 0:128], lhsT=qT.t[:, cc], rhs=Sb.t[:], start=True, stop=True))
                    kb.op('pe', [P2.b, vn.b], [pO2.b], lambda e: e.matmul(pO2.t[0:64, 0:128], lhsT=P2.t[:, c, :], rhs=vn.t[:], start=True, stop=True))
                    kb.op('pe', [kd.b, vn.b], [pS.b], lambda e: e.matmul(pS.t[:, 0:128], lhsT=kd.t[:], rhs=vn.t[:], start=True, stop=True))
                    kb.op('dve', [S.b, Sdec.b, pS.b], [S.b], lambda e: e.scalar_tensor_tensor(out=S.t[:], in0=S.t[:], scalar=Sdec.t[:, c, hd:hd + 1], in1=pS.t[:, 0:128], op0=ALU.mult, op1=ALU.add))
                    kb.op('act', [S.b], [Sb.b], lambda e: e.copy(out=Sb.t[:], in_=S.t[:]))
                    kb.op('act', [pO1.b, egc.b], [t1.b], lambda e: e.activation(out=t1.t[:], in_=pO1.t[0:64, 0:128], func=AF.Copy, scale=egc.t[:, c, hd:hd + 1]))
                    if d == 0:
                        kb.op('dve', [t1.b, pO2.b], [o_all.b], lambda e: e.tensor_tensor(out=o_all.t[:, c, :], in0=t1.t[:], in1=pO2.t[0:64, 0:128], op=ALU.add))
                    else:
                        t2 = t2r.next()
                        kb.op('dve', [t1.b, pO2.b], [t2.b], lambda e: e.tensor_tensor(out=t2.t[:], in0=t1.t[:], in1=pO2.t[0:64, 0:128], op=ALU.add))
                        kb.op('dve', [t2.b, o_all.b], [o_all.b], lambda e: e.tensor_tensor(out=o_all.t[:, c, :], in0=o_all.t[:, c, :], in1=t2.t[:], op=ALU.add))
            for pc in range(1 if CUT == 'x' else 4):
                c0 = pc * 17; csl = slice(c0, c0 + 17)
                load(kb, stg, stg.t[:], dr['proj'][c0 * 64:(c0 + 17) * 64, O_AZ + h * 128:O_AZ + (h + 1) * 128].rearrange("(c p) d -> p c d", p=64))
                kb.op('act', [stg.b], [stg.b], lambda e: e.activation(out=stg.t[:], in_=stg.t[:], func=AF.Silu))
                for g0 in range(0, 17, 8):
                    n = min(8, 17 - g0); gs_ = slice(c0 + g0, c0 + g0 + n)
                    sqv = Xs.t[:, 0:n, :]
                    kb.op('dve', [o_all.b], [Xs.b], lambda e: e.tensor_tensor(out=sqv, in0=o_all.t[:, gs_, 0:64], in1=o_all.t[:, gs_, 0:64], op=ALU.mult))
                    kb.op('dve', [o_all.b], [Xp.b], lambda e: e.tensor_tensor(out=Xp.t[:, 0:n, :], in0=o_all.t[:, gs_, 64:128], in1=o_all.t[:, gs_, 64:128], op=ALU.mult))
                    kb.op('dve', [Xs.b, Xp.b], [Xs.b], lambda e: e.tensor_tensor(out=sqv, in0=sqv, in1=Xp.t[:, 0:n, :], op=ALU.add))
                    kb.op('dve', [Xs.b], [rs17.b], lambda e: e.reduce_sum(out=rs17.t[:, g0:g0 + n], in_=sqv, axis=AX.X))
                kb.op('dve', [rs17.b], [rs17.b], lambda e: e.tensor_scalar(out=rs17.t[:], in0=rs17.t[:], scalar1=1.0 / 128, scalar2=EPS, op0=ALU.mult, op1=ALU.add))
                kb.op('act', [rs17.b], [rs17.b], lambda e: e.sqrt(out=rs17.t[:], in_=rs17.t[:]))
                kb.op('dve', [rs17.b], [rs17.b], lambda e: e.reciprocal(out=rs17.t[:], in_=rs17.t[:]))
                kb.op('dve', [o_all.b, rs17.b], [o_all.b], lambda e: e.tensor_tensor(out=o_all.t[:, csl, :], in0=o_all.t[:, csl, :], in1=rs17.t[:].unsqueeze(2).to_broadcast([64, 17, 128]), op=ALU.mult))
                kb.op('dve', [o_all.b, gg.b], [o_all.b], lambda e: e.tensor_tensor(out=o_all.t[:, csl, :], in0=o_all.t[:, csl, :], in1=gg.t[:].unsqueeze(1).to_broadcast([64, 17, 128]), op=ALU.mult))
                kb.op('dve', [o_all.b, stg.b], [o_all.b], lambda e: e.tensor_tensor(out=o_all.t[:, csl, :], in0=o_all.t[:, csl, :], in1=stg.t[:], op=ALU.mult))
                store(kb, o_all, dr['ybr'][c0 * 64:(c0 + 17) * 64, h * 128:(h + 1) * 128].rearrange("(c p) d -> p c d", p=64), o_all.t[:, csl, :], eng='sp')


def load_bf16_weight(kb, ph, dst, dst_chunks, src_chunk_aps, stg):
    for i, ap in enumerate(src_chunk_aps):
        s_ = stg.next()
        n = ap.shape[-1]
        load(kb, s_, s_.t[:, :n], ap)
        kb.op('pool', [s_.b], [dst.b], lambda e: e.tensor_copy(out=dst.t[:, i, :n], in_=s_.t[:, :n]))


def phase_merge(kb, dr, l, ident, ctx_out):
    ntile = NT if ctx_out else 32
    if CUT:
        ntile = 1
    with Phase(kb) as ph:
        stg = ph.ring(2, [128, 1024], F32, dma=True)
        wup = ph.sb([128, 12, 1024], BF16); wout = ph.sb([128, 8, 1024], BF16)
        load_bf16_weight(kb, ph, wup, 12, [dr['w_up'][l, i // 4, (i % 4) * 128:(i % 4 + 1) * 128, :] for i in range(12)], stg)
        load_bf16_weight(kb, ph, wout, 8, [dr['w_out'][l, i * 128:(i + 1) * 128, :] for i in range(8)], stg)
        gts = []
        for r in range(2):
            g = ph.sb([128, D], F32, dma=True)
            load(kb, g, g.t[:], dr['mod'][l, r, 2 * D:3 * D].partition_broadcast(128))
            gts.append(g)
        yr = ph.ring(2, [128, 1536], F32, dma=True); ybr_ = ph.ring(2, [128, 1536], BF16)
        gr = ph.ring(2, [128, 3, 1024], F32, dma=True)
        xr = ph.ring(2, [128, D], F32, dma=True)
        yT = ph.sb([128, 12, 128], BF16); mT = ph.sb([128, 8, 128], BF16)
        t0 = ph.sb([128, 512], F32); t1 = ph.sb([128, 512], F32); mb = ph.sb([128, 1024], BF16)
        ptY = ph.ps([128, 12, 128], BF16); ptM = ph.ps([128, 8, 128], BF16)
        upr = ph.ring(3, [128, 512], F32, psum=True); o2r = ph.ring(2, [128, 512], F32, psum=True)
        for t in range(ntile):
            rows = slice(t * 128, (t + 1) * 128)
            y = yr.next(); yb = ybr_.next(); g = gr.next(); xt = xr.next()
            load(kb, y, y.t[:], dr['ybr'][rows, :])
            load(kb, g, g.t[:], dr['proj'][rows, O_G:O_G + 3072].rearrange("p (n c) -> p n c", n=3))
            load(kb, xt, xt.t[:], x_src(dr, l, t))
            kb.op('dve', [y.b], [yb.b], lambda e: e.tensor_copy(out=yb.t[:], in_=y.t[:]))
            kb.op('act', [g.b], [g.b], lambda e: e.activation(out=g.t[:], in_=g.t[:], func=AF.Sigmoid))
            for k in range(12):
                kb.op('pe', [yb.b, ident.b], [ptY.b], lambda e: e.transpose(out=ptY.t[:, k, :], in_=yb.t[:, k * 128:(k + 1) * 128], identity=ident.t[:]))
            kb.op('act', [ptY.b], [yT.b], lambda e: e.copy(out=yT.t[:], in_=ptY.t[:]))
            for half in range(2):
                cs = slice(half * 512, (half + 1) * 512)
                ups = []
                for n in range(3):
                    up = upr.next(); ups.append(up)
                    for kc in range(4):
                        kb.op('pe', [yT.b, wup.b], [up.b], lambda e: e.matmul(up.t[:], lhsT=yT.t[:, n * 4 + kc, :], rhs=wup.t[:, n * 4 + kc, cs], start=(kc == 0), stop=(kc == 3)))
                kb.op('dve', [ups[0].b, g.b], [t0.b], lambda e: e.tensor_tensor(out=t0.t[:], in0=ups[0].t[:], in1=g.t[:, 0, cs], op=ALU.mult))
                kb.op('dve', [ups[1].b, g.b], [t1.b], lambda e: e.tensor_tensor(out=t1.t[:], in0=ups[1].t[:], in1=g.t[:, 1, cs], op=ALU.mult))
                kb.op('dve', [t0.b, t1.b], [t0.b], lambda e: e.tensor_tensor(out=t0.t[:], in0=t0.t[:], in1=t1.t[:], op=ALU.add))
                kb.op('dve', [ups[2].b, g.b], [t1.b], lambda e: e.tensor_tensor(out=t1.t[:], in0=ups[2].t[:], in1=g.t[:, 2, cs], op=ALU.mult))
                kb.op('dve', [t0.b, t1.b], [mb.b], lambda e: e.tensor_tensor(out=mb.t[:, cs], in0=t0.t[:], in1=t1.t[:], op=ALU.add))
            for k in range(8):
                kb.op('pe', [mb.b, ident.b], [ptM.b], lambda e: e.transpose(out=ptM.t[:, k, :], in_=mb.t[:, k * 128:(k + 1) * 128], identity=ident.t[:]))
            kb.op('act', [ptM.b], [mT.b], lambda e: e.copy(out=mT.t[:], in_=ptM.t[:]))
            gt = gts[0] if t < 32 else gts[1]
            for half in range(2):
                cs = slice(half * 512, (half + 1) * 512)
                o2 = o2r.next()
                for kc in range(8):
                    kb.op('pe', [mT.b, wout.b], [o2.b], lambda e: e.matmul(o2.t[:], lhsT=mT.t[:, kc, :], rhs=wout.t[:, kc, cs], start=(kc == 0), stop=(kc == 7)))
                kb.op('dve', [o2.b, gt.b], [t0.b], lambda e: e.tensor_tensor(out=t0.t[:], in0=o2.t[:], in1=gt.t[:, cs], op=ALU.mult))
                kb.op('dve', [t0.b, xt.b], [xt.b], lambda e: e.tensor_tensor(out=xt.t[:, cs], in0=xt.t[:, cs], in1=t0.t[:], op=ALU.add))
            store(kb, xt, dr['xres'][rows, :], xt.t[:], eng='sp')


def top16(kb, src_tile, src_ap, work, vals_tile, vals_ap, idx_tile, idx_ap):
    kb.op('dve', [src_tile.b], [vals_tile.b], lambda e: e.max(out=vals_ap[:, 0:8], in_=src_ap))
    kb.op('dve', [src_tile.b, vals_tile.b], [idx_tile.b], lambda e: e.max_index(out=idx_ap[:, 0:8], in_max=vals_ap[:, 0:8], in_values=src_ap))
    kb.op('dve', [src_tile.b, vals_tile.b], [work.b], lambda e: e.match_replace(out=work.t[:, :src_ap.shape[-1]], in_to_replace=vals_ap[:, 0:8], in_values=src_ap, imm_value=-1e30))
    wk = work.t[:, :src_ap.shape[-1]]
    kb.op('dve', [work.b], [vals_tile.b], lambda e: e.max(out=vals_ap[:, 8:16], in_=wk))
    kb.op('dve', [work.b, vals_tile.b], [idx_tile.b], lambda e: e.max_index(out=idx_ap[:, 8:16], in_max=vals_ap[:, 8:16], in_values=wk))


def phase_peer(kb, dr, l, ident, ctx_out):
    last = (l == DEPTH - 1)
    ntile = NT if ctx_out else 32
    if CUT:
        ntile = 1
    with Phase(kb) as ph:
        stg = ph.ring(2, [128, 2048], F32, dma=True)
        wq = ph.sb([128, 8, 2048], BF16)
        load_bf16_weight(kb, ph, wq, 8, [dr['peer_wq'][l, i * 128:(i + 1) * 128, :] for i in range(8)], stg)
        keysT = ph.sb([128, 16, 128], BF16)
        ptk = ph.ps([128, 4, 128], BF16)
        for g4 in range(4):
            s_ = stg.next()
            load(kb, s_, s_.t[:, 0:512].rearrange("p (j d) -> p j d", j=4), dr['peer_keys'][l].rearrange("h p n d -> n (h p) d")[:, g4 * 4:(g4 + 1) * 4, :])
            kbf = ph.sb([128, 512], BF16)
            kb.op('dve', [s_.b], [kbf.b], lambda e: e.tensor_copy(out=kbf.t[:], in_=s_.t[:, 0:512]))
            for j in range(4):
                kb.op('pe', [kbf.b, ident.b], [ptk.b], lambda e: e.transpose(out=ptk.t[:, j, :], in_=kbf.t[:, j * 128:(j + 1) * 128], identity=ident.t[:]))
            kb.op('act', [ptk.b], [keysT.b], lambda e: e.copy(out=keysT.t[:, g4 * 4:(g4 + 1) * 4, :], in_=ptk.t[:]))
        AB = load_modAB(kb, ph, dr, l, 1, 'norm2_g')
        gts = []
        for r in range(2):
            g = ph.sb([128, D], F32, dma=True)
            load(kb, g, g.t[:], dr['mod'][l, r, 5 * D:6 * D].partition_broadcast(128))
            gts.append(g)
        iota = ph.sb([128, 16], F32, dma=True)
        load(kb, iota, iota.t[:], dr['iota16'])
        thr = ph.sb([128, 16], F32)
        kb.op('dve', [iota.b], [thr.b], lambda e: e.tensor_scalar(out=thr.t[:], in0=iota.t[:], scalar1=16.0, scalar2=None, op0=ALU.mult))
        if last:
            fg = ph.sb([128, D], F32, dma=True)
            load(kb, fg, fg.t[:], dr['final_g'].partition_broadcast(128))
        xr = ph.ring(2, [128, D], F32, dma=True)
        junk = ph.sb([128, D], F32); ss = ph.sb([128, 4], F32)
        h2f = ph.sb([128, D], F32); h2b = ph.sb([128, D], BF16); h2T = ph.sb([128, 8, 128], BF16)
        ptH = ph.ps([128, 8, 128], BF16)
        qpr = ph.ring(2, [128, 4, 128], F32, psum=True); scr = ph.ring(2, [128, 4, 128], F32, psum=True)
        qTb = ph.sb([128, 16, 128], BF16); S = ph.sb([128, 16, 128], F32)
        m16 = ph.sb([128, 16, 16], F32); i16 = ph.sb([128, 16, 16], U32); i16f = ph.sb([128, 16, 16], F32)
        work = ph.sb([128, 256], F32)
        cand = ph.sb([128, 8, 256], F32); eq = ph.sb([128, 8, 16, 16], F32)
        ts = ph.sb([128, 8, 16], F32); pos = ph.sb([128, 8, 16], U32); posf = ph.sb([128, 8, 16], F32)
        pa = ph.sb([128, 8, 16], F32); pb = ph.sb([128, 8, 16], F32)
        i1 = ph.sb([128, 8, 16], F32); i2 = ph.sb([128, 8, 16], F32)
        idx = ph.sb([128, 128], I32); gate = ph.sb([128, 8, 16], F32); gs = ph.sb([128, 8], F32)
        dots = ph.sb([128, 128], F32); wgt = ph.sb([128, 128], F32)
        acc = ph.sb([128, D], F32)
        slots = Ring([ph.sb([128, D], F32, dma=True, sw=True) for _ in range(8)])
        m16v = lambda: m16.t[:].rearrange("p (h two) k -> p h two k", two=2)
        i16v = lambda: i16f.t[:].rearrange("p (h two) k -> p h two k", two=2)
        for t in range(ntile):
            rows = slice(t * 128, (t + 1) * 128)
            xt = xr.next()
            load(kb, xt, xt.t[:], dr['xres'][rows, :])
            A, Bm = AB[0] if t < 32 else AB[1]
            rms_mod_tile(kb, ph, xt, A, Bm, h2f, h2f.t[:], (junk, ss))
            kb.op('act', [h2f.b], [h2b.b], lambda e: e.copy(out=h2b.t[:], in_=h2f.t[:]))
            for k in range(8):
                kb.op('pe', [h2b.b, ident.b], [ptH.b], lambda e: e.transpose(out=ptH.t[:, k, :], in_=h2b.t[:, k * 128:(k + 1) * 128], identity=ident.t[:]))
            kb.op('act', [ptH.b], [h2T.b], lambda e: e.copy(out=h2T.t[:], in_=ptH.t[:]))
            for g4 in range(4):
                qp = qpr.next()
                for j in range(4):
                    hp = g4 * 4 + j
                    for kc in range(8):
                        kb.op('pe', [wq.b, h2T.b], [qp.b], lambda e: e.matmul(qp.t[:, j, :], lhsT=wq.t[:, kc, hp * 128:(hp + 1) * 128], rhs=h2T.t[:, kc, :], start=(kc == 0), stop=(kc == 7)))
                kb.op('act', [qp.b], [qTb.b], lambda e: e.copy(out=qTb.t[:, g4 * 4:(g4 + 1) * 4, :], in_=qp.t[:]))
            for g4 in range(4):
                sc = scr.next()
                for j in range(4):
                    hp = g4 * 4 + j
                    kb.op('pe', [qTb.b, keysT.b], [sc.b], lambda e: e.matmul(sc.t[:, j, :], lhsT=qTb.t[:, hp, :], rhs=keysT.t[:, hp, :], start=True, stop=True))
                kb.op('act', [sc.b], [S.b], lambda e: e.copy(out=S.t[:, g4 * 4:(g4 + 1) * 4, :], in_=sc.t[:]))
            for hp in range(16):
                top16(kb, S, S.t[:, hp, :], work, m16, m16.t[:, hp, :], i16, i16.t[:, hp, :])
            kb.op('dve', [i16.b], [i16f.b], lambda e: e.tensor_copy(out=i16f.t[:], in_=i16.t[:]))
            kb.op('dve', [m16.b], [cand.b], lambda e: e.tensor_tensor(out=cand.t[:].rearrange("p h (a b) -> p h a b", a=16),
                  in0=m16v()[:, :, 0, :].unsqueeze(3).to_broadcast([128, 8, 16, 16]), in1=m16v()[:, :, 1, :].unsqueeze(2).to_broadcast([128, 8, 16, 16]), op=ALU.add))
            for h in range(8):
                top16(kb, cand, cand.t[:, h, :], work, ts, ts.t[:, h, :], pos, pos.t[:, h, :])
            kb.op('dve', [pos.b], [posf.b], lambda e: e.tensor_copy(out=posf.t[:], in_=pos.t[:]))
            kb.op('dve', [posf.b, thr.b], [eq.b], lambda e: e.tensor_tensor(out=eq.t[:], in0=posf.t[:].unsqueeze(3).to_broadcast([128, 8, 16, 16]), in1=thr.t[:].unsqueeze(1).unsqueeze(1).to_broadcast([128, 8, 16, 16]), op=ALU.is_ge))
            kb.op('dve', [eq.b], [pa.b], lambda e: e.reduce_sum(out=pa.t[:], in_=eq.t[:], axis=AX.X))
            kb.op('dve', [pa.b], [pa.b], lambda e: e.tensor_scalar(out=pa.t[:], in0=pa.t[:], scalar1=-1.0, scalar2=None, op0=ALU.add))
            kb.op('dve', [pa.b, posf.b], [pb.b], lambda e: e.scalar_tensor_tensor(out=pb.t[:], in0=pa.t[:], scalar=-16.0, in1=posf.t[:], op0=ALU.mult, op1=ALU.add))
            iob = iota.t[:].unsqueeze(1).unsqueeze(1).to_broadcast([128, 8, 16, 16])
            for (pp, two, dst) in ((pa, 0, i1), (pb, 1, i2)):
                kb.op('dve', [pp.b, iota.b], [eq.b], lambda e: e.tensor_tensor(out=eq.t[:], in0=pp.t[:].unsqueeze(3).to_broadcast([128, 8, 16, 16]), in1=iob, op=ALU.is_equal))
                kb.op('dve', [eq.b, i16f.b], [eq.b], lambda e: e.tensor_tensor(out=eq.t[:], in0=eq.t[:], in1=i16v()[:, :, two, :].unsqueeze(2).to_broadcast([128, 8, 16, 16]), op=ALU.mult))
                kb.op('dve', [eq.b], [dst.b], lambda e: e.reduce_sum(out=dst.t[:], in_=eq.t[:], axis=AX.X))
            kb.op('dve', [i1.b, i2.b], [i1.b], lambda e: e.scalar_tensor_tensor(out=i1.t[:], in0=i1.t[:], scalar=128.0, in1=i2.t[:], op0=ALU.mult, op1=ALU.add))
            kb.op('dve', [i1.b], [idx.b], lambda e: e.tensor_copy(out=idx.t[:], in_=i1.t[:].rearrange("p h k -> p (h k)")))
            kb.op('dve', [ts.b], [gate.b], lambda e: e.tensor_tensor(out=gate.t[:], in0=ts.t[:], in1=ts.t[:, :, 0:1].to_broadcast([128, 8, 16]), op=ALU.subtract))
            kb.op('act', [gate.b], [gate.b], lambda e: e.activation(out=gate.t[:], in_=gate.t[:], func=AF.Exp))
            kb.op('dve', [gate.b], [gs.b], lambda e: e.reduce_sum(out=gs.t[:], in_=gate.t[:], axis=AX.X))
            kb.op('dve', [gs.b], [gs.b], lambda e: e.reciprocal(out=gs.t[:], in_=gs.t[:]))
            kb.op('dve', [gate.b, gs.b], [gate.b], lambda e: e.tensor_tensor(out=gate.t[:], in0=gate.t[:], in1=gs.t[:].unsqueeze(2).to_broadcast([128, 8, 16]), op=ALU.mult))
            for r in range(128):
                sl = slots.next()
                kb.dma('pool', sl.ds, [idx.b], [sl.b], lambda e: e.indirect_dma_start(out=sl.t[:], out_offset=None, in_=dr[f'peer_u{l}'], in_offset=bass.IndirectOffsetOnAxis(ap=idx.t[:, r:r + 1], axis=0)))
                kb.op('dve', [sl.b, h2f.b], [junk.b, dots.b], lambda e: e.scalar_tensor_tensor(out=junk.t[:], in0=sl.t[:], scalar=1.0, in1=h2f.t[:], op0=ALU.mult, op1=ALU.mult, accum_out=dots.t[:, r:r + 1]))
            kb.op('act', [dots.b], [wgt.b], lambda e: e.activation(out=wgt.t[:], in_=dots.t[:], func=AF.Gelu))
            kb.op('dve', [wgt.b, gate.b], [wgt.b], lambda e: e.tensor_tensor(out=wgt.t[:], in0=wgt.t[:], in1=gate.t[:].rearrange("p h k -> p (h k)"), op=ALU.mult))
            for r in range(128):
                sl = slots.next()
                kb.dma('pool', sl.ds, [idx.b], [sl.b], lambda e: e.indirect_dma_start(out=sl.t[:], out_offset=None, in_=dr[f'peer_v{l}'], in_offset=bass.IndirectOffsetOnAxis(ap=idx.t[:, r:r + 1], axis=0)))
                if r == 0:
                    kb.op('dve', [sl.b, wgt.b], [acc.b], lambda e: e.tensor_scalar(out=acc.t[:], in0=sl.t[:], scalar1=wgt.t[:, 0:1], scalar2=None, op0=ALU.mult))
                else:
                    kb.op('dve', [sl.b, wgt.b, acc.b], [acc.b], lambda e: e.scalar_tensor_tensor(out=acc.t[:], in0=sl.t[:], scalar=wgt.t[:, r:r + 1], in1=acc.t[:], op0=ALU.mult, op1=ALU.add))
            gt = gts[0] if t < 32 else gts[1]
            kb.op('dve', [acc.b, gt.b], [acc.b], lambda e: e.tensor_tensor(out=acc.t[:], in0=acc.t[:], in1=gt.t[:], op=ALU.mult))
            kb.op('dve', [acc.b, xt.b], [xt.b], lambda e: e.tensor_tensor(out=xt.t[:], in0=xt.t[:], in1=acc.t[:], op=ALU.add))
            if not last:
                store(kb, xt, dr['xres'][rows, :], xt.t[:], eng='sp')
            else:
                if dr.get('dbg'):
                    store(kb, xt, dr['xres'][rows, :], xt.t[:], eng='sp')
                if t < 32:
                    rms_mod_tile_final(kb, xt, fg, (junk, ss))
                    store(kb, xt, dr['out'][rows, :], xt.t[:], eng='sp')


def rms_mod_tile_final(kb, xt, fg, scratch):
    junk, ss = scratch
    kb.op('act', [xt.b], [junk.b, ss.b], lambda e: e.activation(out=junk.t[:], in_=xt.t[:], func=AF.Square, accum_out=ss.t[:, 0:1]))
    kb.op('dve', [ss.b], [ss.b], lambda e: e.tensor_scalar(out=ss.t[:, 1:2], in0=ss.t[:, 0:1], scalar1=1.0 / D, scalar2=EPS, op0=ALU.mult, op1=ALU.add))
    kb.op('act', [ss.b], [ss.b], lambda e: e.sqrt(out=ss.t[:, 2:3], in_=ss.t[:, 1:2]))
    kb.op('dve', [ss.b], [ss.b], lambda e: e.reciprocal(out=ss.t[:, 3:4], in_=ss.t[:, 2:3]))
    kb.op('dve', [xt.b, ss.b, fg.b], [xt.b], lambda e: e.scalar_tensor_tensor(out=xt.t[:], in0=xt.t[:], scalar=ss.t[:, 3:4], in1=fg.t[:], op0=ALU.mult, op1=ALU.mult))


def w_ext_perm():
    a = np.arange
    o = {'aq': 0, 'ak': 512, 'av': 1024, 'az': 1536, 'b': 2048, 'a': 2056, 'bq': 2064, 'bk': 2576, 'bv': 3088,
         'cq': 3600, 'ck': 4112, 'cv': 4624, 'g': 5136}
    def partner(base):
        idx = []
        for h in range(4):
            for m in range(2):
                for dd in range(64):
                    seg = dd // 32; j = dd % 32
                    pj = j + 16 if j < 16 else j - 16
                    idx.append(base + h * 128 + m * 64 + seg * 32 + pj)
        return np.array(idx)
    cols = np.concatenate([a(0, 2048), a(o['bq'], o['bq'] + 1536), a(o['cq'], o['cq'] + 1536), a(o['g'], o['g'] + 3072),
                           partner(o['bq']), partner(o['bk']), a(2048, 2064)])
    assert cols.shape[0] == NP
    return cols


def build(dbg=False, stop_after=None):
    nc = bass.Bass("TRN2", target_bir_lowering=False)
    dr = {}
    def din(name, shape, dt=F32):
        dr[name] = nc.dram_tensor(name, list(shape), dt, kind="ExternalInput").ap()
    def dscr(name, shape, dt=F32):
        dr[name] = nc.dram_tensor(name, list(shape), dt, kind="ExternalOutput" if dbg else "Internal").ap()
    din('x', [L, D]); din('ctx', [LC, D]); din('c', [D]); din('c_ctx', [D])
    din('norm1_g', [DEPTH, D]); din('norm2_g', [DEPTH, D]); din('ada_w', [DEPTH, D, 6 * D]); din('ada_b', [DEPTH, 6 * D])
    din('w_ext', [DEPTH, D, NP]); din('ident', [128, 128])
    din('rope', [T, 128]); din('diff_lambda', [DEPTH, 4, 64]); din('diff_subln_g', [DEPTH, 128])
    din('na_mask', [64, 64]); din('rpbg', [DEPTH, 8, 64, 15, 64])
    din('gdn_conv', [DEPTH, 5, 1536]); din('gdn_a_log', [DEPTH, 2, 4]); din('gdn_dt_bias', [DEPTH, 2, 4]); din('gdn_norm_g', [DEPTH, 128]); din('gconst', [64, 5, 64])
    din('w_up', [DEPTH, 3, 512, D]); din('w_out', [DEPTH, D, D]); din('peer_wq', [DEPTH, D, 2048]); din('peer_keys', [DEPTH, 8, 2, 128, 128])
    if 'peer' not in SKIP:
        for l_ in range(DEPTH):
            din(f'peer_u{l_}', [16384, D]); din(f'peer_v{l_}', [16384, D])
    din('final_g', [D]); din('iota16', [128, 16])
    if dbg:
        dr['dbg'] = True
    dscr('mod', [DEPTH, 2, 6 * D]); dscr('proj', [T, NP]); dscr('xres', [T, D]); dscr('ybr', [T, 1536])
    dscr('gqkv', [T, 1536]); dscr('gbg', [T, 16])
    dr['out'] = nc.dram_tensor('out', [L, D], F32, kind="ExternalOutput").ap()
    with ExitStack() as es:
        kb = KB(nc, es)
        idf = Tile(kb, es.enter_context(nc.sbuf_tensor('idf', [128, 128], F32)), kb.dsem())
        ident = Tile(kb, es.enter_context(nc.sbuf_tensor('idb', [128, 128], BF16)))
        load(kb, idf, idf.t[:], dr['ident'])
        kb.op('dve', [idf.b], [ident.b], lambda e: e.tensor_copy(out=ident.t[:], in_=idf.t[:]))
        for l in range(DEPTH):
            if 'mod' not in SKIP:
                phase_mod(kb, dr, l)
            if stop_after == ('mod', l): break
            if 'inproj' not in SKIP:
                phase_inproj(kb, dr, l, ident)
            if stop_after == ('inproj', l): break
            ctx_out = l < DEPTH - 1
            if 'gdn' not in SKIP:
                phase_gdn_prep(kb, dr, l)
                if stop_after == ('gdnprep', l): break
                phase_gdn(kb, dr, l, ident)
            if stop_after == ('gdn', l): break
            if 'diff' not in SKIP:
                phase_diff(kb, dr, l, ident, ctx_out)
            if stop_after == ('diff', l): break
            if 'natten' not in SKIP:
                phase_natten(kb, dr, l, ident, ctx_out)
            if stop_after == ('natten', l): break
            if 'merge' not in SKIP:
                phase_merge(kb, dr, l, ident, ctx_out)
            if stop_after == ('merge', l): break
            if 'peer' not in SKIP:
                phase_peer(kb, dr, l, ident, ctx_out)
            if stop_after == ('peer', l): break
        kb.barrier()
        print('instructions:', kb.ninst)
    return nc


def host_consts():
    t = np.arange(L)
    row, colp = t // 64, t % 64
    inv = 1.0 / (10000.0 ** (np.arange(16, dtype=np.float32) / 16))
    cos = np.ones((T, 64), np.float32); sin = np.zeros((T, 64), np.float32)
    for seg, pos in ((0, row), (1, colp)):
        ang = pos.astype(np.float32)[:, None] * inv[None, :]
        c, s_ = np.cos(ang), np.sin(ang)
        cos[:L, seg * 32:seg * 32 + 16] = c; cos[:L, seg * 32 + 16:seg * 32 + 32] = c
        sin[:L, seg * 32:seg * 32 + 16] = -s_; sin[:L, seg * 32 + 16:seg * 32 + 32] = s_
    col = np.arange(64)
    cs = np.clip(col - 8, 0, 48)
    ok = (col[None, :] >= cs[:, None]) & (col[None, :] < cs[:, None] + 16)
    mask = np.where(ok.T, 0.0, -30000.0).astype(np.float32)
    ii = np.arange(64)
    Lf = (ii[:, None] <= ii[None, :]).astype(np.float32)
    Lb = (ii[:, None] >= ii[None, :]).astype(np.float32)
    Sf = (ii[None, :] < ii[:, None]).astype(np.float32)
    Sb_ = (ii[None, :] > ii[:, None]).astype(np.float32)
    gconst = np.stack([Lf, Lb, Sf, Sb_, np.eye(64, dtype=np.float32)], axis=1)
    return {'rope': np.concatenate([cos, sin], 1).astype(np.float32), 'na_mask': mask, 'gconst': np.ascontiguousarray(gconst)}


def make_in_maps(inputs):
    perm = w_ext_perm()
    w_ext = np.ascontiguousarray(inputs['w_in'][:, :, perm])
    shared = {k: np.ascontiguousarray(inputs[k]) for k in ('c_ctx', 'norm1_g', 'norm2_g', 'ada_w', 'ada_b')}
    shared['w_ext'] = w_ext
    shared['ident'] = np.eye(128, dtype=np.float32)
    shared.update(host_consts())
    shared['iota16'] = np.tile(np.arange(16, dtype=np.float32)[None, :], (128, 1))
    for k in ('gdn_conv', 'gdn_a_log', 'gdn_dt_bias', 'gdn_norm_g', 'diff_lambda', 'diff_subln_g', 'w_up', 'w_out', 'peer_wq', 'peer_keys', 'final_g'):
        shared[k] = np.ascontiguousarray(inputs[k])
    for l_ in range(DEPTH):
        shared[f'peer_u{l_}'] = np.ascontiguousarray(inputs['peer_u'][l_]); shared[f'peer_v{l_}'] = np.ascontiguousarray(inputs['peer_v'][l_])
    col = np.arange(64)
    dc = np.clip(col[None, :] - col[:, None], -15, 15) + 15
    rp = inputs['na_rpb']
    shared['rpbg'] = np.ascontiguousarray(rp[:, :, :, dc.T].transpose(0, 1, 3, 2, 4))
    maps = []
    for c in range(8):
        b = c % 4
        m = dict(shared)
        m['x'] = np.ascontiguousarray(inputs['x'][b]); m['ctx'] = np.ascontiguousarray(inputs['ctx'][b])
        m['c'] = np.ascontiguousarray(inputs['c'][b])
        maps.append(m)
    return maps


def kernel(**inputs):
    inputs = {k: np.asarray(v) for k, v in inputs.items()}
    nc = build()
    maps = make_in_maps(inputs)
    if 'peer' in SKIP:
        maps = [{k: v for k, v in m.items() if not k.startswith(('peer_u', 'peer_v'))} for m in maps]
    res = run_bass_kernel_spmd(nc, maps, core_ids=list(range(8)))
    return np.stack([res.results[b]['out'] for b in range(4)], axis=0)
```

```python
import math
import numpy as np
from contextlib import ExitStack
import concourse.bass as bass
import concourse.mybir as mybir
from concourse.bass_utils import run_bass_kernel_spmd

F32 = mybir.dt.float32; BF16 = mybir.dt.bfloat16; I32 = mybir.dt.int32; U32 = mybir.dt.uint32
AF = mybir.ActivationFunctionType; ALU = mybir.AluOpType; AX = mybir.AxisListType

D = 1024; L = 4096; LC = 256; T = L + LC; NT = T // 128; DEPTH = 2
NP = 9232
O_AQ, O_AK, O_AV, O_AZ, O_BQ, O_BK, O_BV, O_CQ, O_CK, O_CV, O_G, O_BQP, O_BKP, O_BA = (
    0, 512, 1024, 1536, 2048, 2560, 3072, 3584, 4096, 4608, 5120, 8192, 8704, 9216)
EPS = 1e-6
SAME_ENGINE_SYNC = True
SKIP = set()
SEM_LIMIT = 6000
CUT = None
NAT_ROWS = 8


class Buf:
    __slots__ = ('name', 'w', 'r')

    def __init__(self, name):
        self.name = name; self.w = None; self.r = []


class DSem:
    __slots__ = ('sem', 'cnt', 'sw')

    def __init__(self, sem):
        self.sem = sem; self.cnt = 0; self.sw = False


class KB:
    def __init__(self, nc, es):
        self.nc = nc; self.es = es
        self.eng = {'pe': nc.tensor, 'act': nc.scalar, 'dve': nc.vector, 'pool': nc.gpsimd, 'sp': nc.sync}
        self.esem = {e: es.enter_context(nc.semaphore('se_' + e)) for e in ('pe', 'act', 'dve', 'pool')}
        self.ecnt = {e: 0 for e in self.esem}
        self.own_nums = {e: {self.esem[e].num} for e in self.esem}
        self.seen = {e: {} for e in self.eng}
        self.free_dsems = []
        self.free_dsems_sw = []
        self.all_dsems = []
        self.phase_sem = es.enter_context(nc.semaphore('phase'))
        self.phase_no = 0
        self.nbuf = 0
        self.ninst = 0
        self.all_bufs = []
        self.nrot = 0
        self.retired = []

    def buf(self, name=None):
        self.nbuf += 1
        b = Buf(name or f'b{self.nbuf}')
        self.all_bufs.append(b)
        return b

    def dsem(self, sw=False):
        if sw:
            if self.free_dsems_sw:
                return self.free_dsems_sw.pop()
        elif self.free_dsems:
            return self.free_dsems.pop()
        d = DSem(self.es.enter_context(self.nc.semaphore(f'ds{len(self.all_dsems)}')))
        self.all_dsems.append(d)
        return d

    def _wait(self, e, tok):
        sem, val = tok
        key = sem.num
        if self.seen[e].get(key, 0) >= val:
            return
        self.eng[e].wait_ge(sem, val)
        self.seen[e][key] = val

    def _deps(self, e, reads, writes):
        skip_own = (e == 'pe') or not SAME_ENGINE_SYNC
        own = self.own_nums[e] if e in self.own_nums else ()
        for b in reads:
            if b.w is not None and not (skip_own and b.w[0].num in own):
                self._wait(e, b.w)
        for b in writes:
            if b.w is not None and not (skip_own and b.w[0].num in own):
                self._wait(e, b.w)
            for t in b.r:
                if not (skip_own and t[0].num in own):
                    self._wait(e, t)

    def _fresh_sem(self):
        self.nrot += 1
        return self.es.enter_context(self.nc.semaphore(f'rot{self.nrot}'))

    def op(self, e, reads, writes, fn):
        if self.ecnt[e] >= SEM_LIMIT:
            self.esem[e] = self._fresh_sem(); self.ecnt[e] = 0
            self.own_nums[e].add(self.esem[e].num)
        self._deps(e, reads, writes)
        ins = fn(self.eng[e])
        self.ecnt[e] += 1
        self.ninst += 1
        ins.then_inc(self.esem[e], 1)
        tok = (self.esem[e], self.ecnt[e])
        for b in reads:
            b.r.append(tok)
        for b in writes:
            b.w = tok; b.r = []
        return ins

    def dma(self, e, ds, reads, writes, fn):
        if ds.cnt >= SEM_LIMIT:
            self.retired.append((ds.sem, ds.cnt))
            ds.sem = self._fresh_sem(); ds.cnt = 0
        self._deps(e, reads, writes)
        ins = fn(self.eng[e])
        self.ninst += 1
        ds.cnt += 16
        ins.then_inc(ds.sem, 16)
        tok = (ds.sem, ds.cnt)
        for b in reads:
            b.r.append(tok)
        for b in writes:
            b.w = tok; b.r = []
        return ins

    def barrier(self, release=()):
        for e in self.esem:
            if self.ecnt[e] > 0:
                self._wait('sp', (self.esem[e], self.ecnt[e]))
        for d in self.all_dsems:
            if d.cnt > 0:
                self._wait('sp', (d.sem, d.cnt))
        for tok in self.retired:
            self._wait('sp', tok)
        self.retired = []
        self.phase_no += 1
        self.nc.sync.sem_inc(self.phase_sem, 1)
        for e in ('pe', 'act', 'dve', 'pool'):
            self.eng[e].wait_ge(self.phase_sem, self.phase_no)
        for d in release:
            (self.free_dsems_sw if getattr(d, 'sw', False) else self.free_dsems).append(d)


class Tile:
    def __init__(self, kb, t, ds=None):
        self.t = t; self.b = kb.buf(); self.ds = ds


class Phase:
    def __init__(self, kb):
        self.kb = kb; self.nc = kb.nc; self.es = ExitStack(); self.dsems = []; self.n = 0

    def __enter__(self):
        self.es.__enter__(); return self

    def __exit__(self, *a):
        self.kb.barrier(release=self.dsems)
        return self.es.__exit__(*a)

    def sb(self, shape, dt, dma=False, sw=False):
        self.n += 1
        t = self.es.enter_context(self.nc.sbuf_tensor(f'p{self.kb.phase_no}_s{self.n}', list(shape), dt))
        ds = None
        if dma:
            ds = self.kb.dsem(sw=sw); ds.sw = sw; self.dsems.append(ds)
        return Tile(self.kb, t, ds)

    def ps(self, shape, dt=F32):
        self.n += 1
        t = self.es.enter_context(self.nc.psum_tensor(f'p{self.kb.phase_no}_q{self.n}', list(shape), dt))
        return Tile(self.kb, t)

    def ring(self, n, shape, dt, dma=False, psum=False):
        return Ring([self.ps(shape, dt) if psum else self.sb(shape, dt, dma) for _ in range(n)])


class Ring:
    def __init__(self, tiles):
        self.tiles = tiles; self.i = 0

    def next(self):
        t = self.tiles[self.i % len(self.tiles)]; self.i += 1
        return t


def load(kb, tl, dst_ap, src_ap, eng='sp', slow=False):
    return kb.dma(eng, tl.ds, [], [tl.b], lambda e: e.dma_start(out=dst_ap, in_=src_ap, allow_slow_non_contiguous=slow))


def store(kb, tl, dst_ap, src_ap, eng='sp'):
    return kb.dma(eng, tl.ds, [tl.b], [], lambda e: e.dma_start(out=dst_ap, in_=src_ap))


def tok_rows(dr, t):
    return dr[t * 128:(t + 1) * 128]


def phase_mod(kb, dr, l):
    with Phase(kb) as ph:
        cc = ph.sb([128, 8, 2], F32, dma=True)
        cb = ph.sb([128, 8, 2], BF16)
        ab = ph.sb([2, 6144], F32, dma=True)
        mo = ph.sb([2, 6144], F32, dma=True)
        wr = ph.ring(2, [128, 8, 512], F32, dma=True)
        wbr = ph.ring(2, [128, 8, 512], BF16)
        pr = ph.ring(2, [2, 512], F32, psum=True)
        load(kb, cc, cc.t[:, :, 0], dr['c'].rearrange("(k p) -> p k", p=128), slow=True)
        load(kb, cc, cc.t[:, :, 1], dr['c_ctx'].rearrange("(k p) -> p k", p=128), slow=True)
        load(kb, ab, ab.t[0:1, :], dr['ada_b'][l:l + 1, :])
        load(kb, ab, ab.t[1:2, :], dr['ada_b'][l:l + 1, :])
        kb.op('act', [cc.b], [cb.b], lambda e: e.activation(out=cb.t[:], in_=cc.t[:], func=AF.Silu))
        for nb in range(12):
            w = wr.next(); wb = wbr.next(); p = pr.next()
            load(kb, w, w.t[:], dr['ada_w'][l, :, nb * 512:(nb + 1) * 512].rearrange("(k p) n -> p k n", p=128))
            kb.op('pool', [w.b], [wb.b], lambda e: e.tensor_copy(out=wb.t[:], in_=w.t[:]))
            for k in range(8):
                kb.op('pe', [cb.b, wb.b], [p.b], lambda e: e.matmul(p.t[:], lhsT=cb.t[:, k, :], rhs=wb.t[:, k, :], start=(k == 0), stop=(k == 7)))
            kb.op('dve', [p.b, ab.b], [mo.b], lambda e: e.tensor_tensor(out=mo.t[:, nb * 512:(nb + 1) * 512], in0=p.t[:], in1=ab.t[:, nb * 512:(nb + 1) * 512], op=ALU.add))
        store(kb, mo, dr['mod'][l], mo.t[:])


def bcast_rows(ap_row, n=128):
    return ap_row.partition_broadcast(n)


def rms_mod_tile(kb, ph, xt, A, Bm, out_tile, out_ap, scratch):
    junk, ss = scratch
    kb.op('act', [xt.b], [junk.b, ss.b], lambda e: e.activation(out=junk.t[:], in_=xt.t[:], func=AF.Square, accum_out=ss.t[:, 0:1]))
    kb.op('dve', [ss.b], [ss.b], lambda e: e.tensor_scalar(out=ss.t[:, 1:2], in0=ss.t[:, 0:1], scalar1=1.0 / D, scalar2=EPS, op0=ALU.mult, op1=ALU.add))
    kb.op('act', [ss.b], [ss.b], lambda e: e.sqrt(out=ss.t[:, 2:3], in_=ss.t[:, 1:2]))
    kb.op('dve', [ss.b], [ss.b], lambda e: e.reciprocal(out=ss.t[:, 3:4], in_=ss.t[:, 2:3]))
    kb.op('dve', [xt.b, ss.b, A.b], [junk.b], lambda e: e.scalar_tensor_tensor(out=junk.t[:], in0=xt.t[:], scalar=ss.t[:, 3:4], in1=A.t[:], op0=ALU.mult, op1=ALU.mult))
    kb.op('dve', [junk.b, Bm.b], [out_tile.b], lambda e: e.tensor_tensor(out=out_ap, in0=junk.t[:], in1=Bm.t[:], op=ALU.add))


def load_modAB(kb, ph, dr, l, which, g_name):
    res = []
    g = ph.sb([128, D], F32, dma=True)
    load(kb, g, g.t[:], bcast_rows(dr[g_name][l:l + 1, :]))
    for r in range(2):
        A = ph.sb([128, D], F32, dma=True); Bm = ph.sb([128, D], F32, dma=True)
        o = which * 3 * D
        load(kb, Bm, Bm.t[:], bcast_rows(dr['mod'][l, r:r + 1, o:o + D]))
        load(kb, A, A.t[:], bcast_rows(dr['mod'][l, r:r + 1, o + D:o + 2 * D]))
        kb.op('dve', [A.b, g.b], [A.b], lambda e: e.scalar_tensor_tensor(out=A.t[:], in0=A.t[:], scalar=1.0, in1=g.t[:], op0=ALU.add, op1=ALU.mult))
        res.append((A, Bm))
    return res


def x_src(dr, l, t):
    if l == 0:
        return dr['x'][t * 128:(t + 1) * 128] if t < 32 else dr['ctx'][(t - 32) * 128:(t - 31) * 128]
    return dr['xres'][t * 128:(t + 1) * 128]


def phase_inproj(kb, dr, l, ident):
    with Phase(kb) as ph:
        hT = ph.sb([128, 8, T], BF16)
        AB = load_modAB(kb, ph, dr, l, 0, 'norm1_g')
        xr = ph.ring(2, [128, D], F32, dma=True)
        junk = ph.sb([128, D], F32); ss = ph.sb([128, 4], F32)
        hbr = ph.ring(2, [128, D], BF16)
        ptr = ph.ring(2, [128, 8, 128], BF16, psum=True)
        for t in range(NT):
            xt = xr.next(); hb = hbr.next(); pt = ptr.next()
            load(kb, xt, xt.t[:], x_src(dr, l, t))
            A, Bm = AB[0] if t < 32 else AB[1]
            rms_mod_tile(kb, ph, xt, A, Bm, hb, hb.t[:], (junk, ss))
            for k in range(8):
                kb.op('pe', [hb.b, ident.b], [pt.b], lambda e: e.transpose(out=pt.t[:, k, :], in_=hb.t[:, k * 128:(k + 1) * 128], identity=ident.t[:]))
            kb.op('act', [pt.b], [hT.b], lambda e: e.copy(out=hT.t[:, :, t * 128:(t + 1) * 128], in_=pt.t[:]))
        wr = ph.ring(2, [128, 8, 512], F32, dma=True)
        wbr = ph.ring(2, [128, 8, 512], BF16)
        pr = ph.ring(4, [128, 512], F32, psum=True)
        orr = ph.ring(4, [128, 512], F32, dma=True)
        nblk = (NP + 511) // 512
        i = 0
        for cbk in range(nblk):
            c0 = cbk * 512; nc_ = min(512, NP - c0)
            w = wr.next(); wb = wbr.next()
            load(kb, w, w.t[:, :, :nc_], dr['w_ext'][l, :, c0:c0 + nc_].rearrange("(k p) n -> p k n", p=128))
            kb.op('pool', [w.b], [wb.b], lambda e: e.tensor_copy(out=wb.t[:, :, :nc_], in_=w.t[:, :, :nc_]))
            for t in range(NT):
                p = pr.next(); o = orr.next()
                for k in range(8):
                    kb.op('pe', [hT.b, wb.b], [p.b], lambda e: e.matmul(p.t[:, :nc_], lhsT=hT.t[:, k, t * 128:(t + 1) * 128], rhs=wb.t[:, k, :nc_], start=(k == 0), stop=(k == 7)))
                if i % 2 == 0:
                    kb.op('act', [p.b], [o.b], lambda e: e.copy(out=o.t[:, :nc_], in_=p.t[:, :nc_]))
                else:
                    kb.op('dve', [p.b], [o.b], lambda e: e.tensor_copy(out=o.t[:, :nc_], in_=p.t[:, :nc_]))
                i += 1
                store(kb, o, dr['proj'][t * 128:(t + 1) * 128, c0:c0 + nc_], o.t[:, :nc_])


def phase_diff(kb, dr, l, ident, ctx_out):
    lam_init = 0.8 - 0.6 * math.exp(-0.3 * l)
    with Phase(kb) as ph:
        rope = ph.sb([128, NT, 128], F32, dma=True)
        load(kb, rope, rope.t[:], dr['rope'].rearrange("(t p) c -> p t c", p=128))
        lp = ph.sb([128, 4, 64], F32, dma=True)
        load(kb, lp, lp.t[:], dr['diff_lambda'][l].partition_broadcast(128))
        lw = ph.sb([128, 2, 64], F32); ls = ph.sb([128, 4], F32)
        kb.op('dve', [lp.b], [lw.b], lambda e: e.tensor_tensor(out=lw.t[:, 0, :], in0=lp.t[:, 0, :], in1=lp.t[:, 1, :], op=ALU.mult))
        kb.op('dve', [lp.b], [lw.b], lambda e: e.tensor_tensor(out=lw.t[:, 1, :], in0=lp.t[:, 2, :], in1=lp.t[:, 3, :], op=ALU.mult))
        kb.op('dve', [lw.b], [ls.b], lambda e: e.reduce_sum(out=ls.t[:, 0:2], in_=lw.t[:], axis=AX.X))
        kb.op('act', [ls.b], [ls.b], lambda e: e.activation(out=ls.t[:, 2:4], in_=ls.t[:, 0:2], func=AF.Exp))
        kb.op('dve', [ls.b], [ls.b], lambda e: e.tensor_tensor(out=ls.t[:, 0:1], in0=ls.t[:, 3:4], in1=ls.t[:, 2:3], op=ALU.subtract))
        kb.op('dve', [ls.b], [ls.b], lambda e: e.tensor_scalar(out=ls.t[:, 1:2], in0=ls.t[:, 0:1], scalar1=-lam_init, scalar2=None, op0=ALU.add))
        gsub = ph.sb([128, 128], F32, dma=True)
        load(kb, gsub, gsub.t[:], dr['diff_subln_g'][l].partition_broadcast(128))
        kb.op('dve', [gsub.b], [gsub.b], lambda e: e.tensor_scalar(out=gsub.t[:], in0=gsub.t[:], scalar1=1.0 - lam_init, scalar2=None, op0=ALU.mult))
        qT = ph.sb([64, 2, T], BF16); kT = ph.sb([64, 2, T], BF16); vaug = ph.sb([128, NT, 130], BF16)
        kb.op('dve', [], [vaug.b], lambda e: e.memset(vaug.t[:, :, 128:130], 1.0))
        stg = ph.ring(2, [128, 5, 128], F32, dma=True)
        tmp = ph.ring(2, [128, 4, 128], F32)
        qk = ph.ring(2, [128, 2, 128], BF16)
        ptr = ph.ring(1, [64, 4, 128], BF16, psum=True)
        STr = ph.ring(2, [128, 512], F32, psum=True)
        Or = ph.ring(1, [128, 4, 512], F32, psum=True)
        PTr = ph.ring(3, [128, 512], BF16)
        om = ph.sb([128, 2, 4, 128], F32); rec = ph.sb([128, 8], F32)
        ob = ph.ring(2, [128, 4, 128], F32, dma=True); junk = ph.sb([128, 4, 128], F32)
        for h in range(1 if CUT else 4):
            for t in range(0 if CUT == 'diff_prep0' else NT):
                sg = stg.next(); tm = tmp.next(); qb_ = qk.next(); pt = ptr.next()
                rows = slice(t * 128, (t + 1) * 128)
                for j, off in enumerate((O_BQ, O_BQP, O_BK, O_BKP, O_BV)):
                    load(kb, sg, sg.t[:, j, :], dr['proj'][rows, off + h * 128: off + (h + 1) * 128])
                cosb = rope.t[:, t, 0:64].unsqueeze(1).to_broadcast([128, 2, 64])
                sinb = rope.t[:, t, 64:128].unsqueeze(1).to_broadcast([128, 2, 64])
                v4 = lambda ap: ap.rearrange("p (m d) -> p m d", m=2)
                for j in range(2):
                    kb.op('dve', [sg.b, rope.b], [tm.b], lambda e: e.tensor_tensor(out=v4(tm.t[:, 2 * j, :]), in0=v4(sg.t[:, 2 * j, :]), in1=cosb, op=ALU.mult))
                    kb.op('dve', [sg.b, rope.b], [tm.b], lambda e: e.tensor_tensor(out=v4(tm.t[:, 2 * j + 1, :]), in0=v4(sg.t[:, 2 * j + 1, :]), in1=sinb, op=ALU.mult))
                    kb.op('dve', [tm.b], [qb_.b], lambda e: e.tensor_tensor(out=qb_.t[:, j, :], in0=tm.t[:, 2 * j, :], in1=tm.t[:, 2 * j + 1, :], op=ALU.add))
                kb.op('act', [sg.b], [vaug.b], lambda e: e.copy(out=vaug.t[:, t, 0:128], in_=sg.t[:, 4, :]))
                if CUT == 'diff_prep1':
                    continue
                for j in range(2):
                    for m in range(2):
                        kb.op('pe', [qb_.b, ident.b], [pt.b], lambda e: e.transpose(out=pt.t[:, 2 * j + m, :], in_=qb_.t[:, j, m * 64:(m + 1) * 64], identity=ident.t[:]))
                kb.op('act', [pt.b], [qT.b], lambda e: e.copy(out=qT.t[:, :, rows], in_=pt.t[:, 0:2, :]))
                kb.op('act', [pt.b], [kT.b], lambda e: e.copy(out=kT.t[:, :, rows], in_=pt.t[:, 2:4, :]))
            blocks = [(qb0 * 512, 512, 0, NT) for qb0 in range(8)]
            if ctx_out:
                blocks.append((L, 256, 32, NT))
            if CUT and CUT.startswith('diff_prep'):
                blocks = []
            if CUT == 'diff_b1':
                blocks = blocks[:1]
            for (q0, nq, k0, k1) in blocks:
                ns = nq // 128
                for m in range(2):
                    O = Or.next()
                    for kt in range(k0, k1):
                        ST = STr.next(); PT = PTr.next()
                        kb.op('pe', [kT.b, qT.b], [ST.b], lambda e: e.matmul(ST.t[:, :nq], lhsT=kT.t[:, m, kt * 128:(kt + 1) * 128], rhs=qT.t[:, m, q0:q0 + nq], start=True, stop=True))
                        kb.op('act', [ST.b], [PT.b], lambda e: e.activation(out=PT.t[:, :nq], in_=ST.t[:, :nq], func=AF.Exp, scale=0.125))
                        for qs in range(ns):
                            kb.op('pe', [PT.b, vaug.b], [O.b], lambda e: e.matmul(O.t[:, qs, 0:129], lhsT=PT.t[:, qs * 128:(qs + 1) * 128], rhs=vaug.t[:, kt, 0:129], start=(kt == k0), stop=(kt == k1 - 1)))
                    kb.op('dve', [O.b], [rec.b], lambda e: e.reciprocal(out=rec.t[:, m * 4:m * 4 + ns], in_=O.t[:, 0:ns, 128]))
                    kb.op('dve', [O.b, rec.b], [om.b], lambda e: e.tensor_tensor(out=om.t[:, m, 0:ns, :], in0=O.t[:, 0:ns, 0:128], in1=rec.t[:, m * 4:m * 4 + ns].unsqueeze(2).to_broadcast([128, ns, 128]), op=ALU.mult))
                o = ob.next()
                kb.op('dve', [om.b, ls.b], [o.b], lambda e: e.scalar_tensor_tensor(out=o.t[:, 0:ns, :], in0=om.t[:, 1, 0:ns, :], scalar=ls.t[:, 1:2], in1=om.t[:, 0, 0:ns, :], op0=ALU.mult, op1=ALU.add))
                kb.op('dve', [o.b], [junk.b], lambda e: e.tensor_tensor(out=junk.t[:, 0:ns, :], in0=o.t[:, 0:ns, :], in1=o.t[:, 0:ns, :], op=ALU.mult))
                kb.op('dve', [junk.b], [rec.b], lambda e: e.reduce_sum(out=rec.t[:, 0:ns], in_=junk.t[:, 0:ns, :], axis=AX.X))
                kb.op('dve', [rec.b], [rec.b], lambda e: e.tensor_scalar(out=rec.t[:, 0:ns], in0=rec.t[:, 0:ns], scalar1=1.0 / 128, scalar2=EPS, op0=ALU.mult, op1=ALU.add))
                kb.op('act', [rec.b], [rec.b], lambda e: e.sqrt(out=rec.t[:, 0:ns], in_=rec.t[:, 0:ns]))
                kb.op('dve', [rec.b], [rec.b], lambda e: e.reciprocal(out=rec.t[:, 0:ns], in_=rec.t[:, 0:ns]))
                kb.op('dve', [o.b, rec.b], [o.b], lambda e: e.tensor_tensor(out=o.t[:, 0:ns, :], in0=o.t[:, 0:ns, :], in1=rec.t[:, 0:ns].unsqueeze(2).to_broadcast([128, ns, 128]), op=ALU.mult))
                kb.op('dve', [o.b, gsub.b], [o.b], lambda e: e.tensor_tensor(out=o.t[:, 0:ns, :], in0=o.t[:, 0:ns, :], in1=gsub.t[:].unsqueeze(1).to_broadcast([128, ns, 128]), op=ALU.mult))
                store(kb, o, dr['ybr'][q0:q0 + nq, 512 + h * 128: 512 + (h + 1) * 128].rearrange("(s p) c -> p s c", p=128), o.t[:, 0:ns, :], eng='sp')


def phase_natten(kb, dr, l, ident, ctx_out):
    with Phase(kb) as ph:
        mask = ph.sb([64, 64], F32, dma=True)
        load(kb, mask, mask.t[:], dr['na_mask'])
        bias = ph.sb([64, 15, 64], F32, dma=True)
        qT = ph.sb([64, T], BF16); kT = ph.sb([64, T], BF16); vr = ph.sb([64, 68, 66], BF16)
        kb.op('pool', [], [vr.b], lambda e: e.memset(vr.t[:, :, 64:66], 1.0))
        sq = ph.sb([128, NT, 64], F32, dma=True); sk = ph.sb([128, NT, 64], F32, dma=True); sv = ph.sb([64, 68, 64], F32, dma=True)
        sqb = ph.sb([128, NT, 64], BF16); skb = ph.sb([128, NT, 64], BF16)
        ptr = ph.ring(1, [64, 4, 128], BF16, psum=True)
        Sr = ph.ring(2, [64, 8, 64], F32, psum=True)
        Scr = ph.ring(1, [64, 4, 256], F32, psum=True)
        Or = ph.ring(2, [64, 7, 66], F32, psum=True)
        Sbr = ph.ring(2, [64, 8, 64], F32)
        Pr = ph.ring(2, [64, 8, 64], BF16); Pcr = ph.ring(2, [64, 4, 256], BF16)
        yo = ph.sb([64, 68, 64], F32, dma=True); rec = ph.sb([64, 8], F32)
        for h in range(1 if CUT else 8):
            load(kb, bias, bias.t[:], dr['rpbg'][l, h])
            kb.op('dve', [bias.b, mask.b], [bias.b], lambda e: e.tensor_tensor(out=bias.t[:], in0=bias.t[:], in1=mask.t[:].unsqueeze(1).to_broadcast([64, 15, 64]), op=ALU.add))
            load(kb, sq, sq.t[:], dr['proj'][:, O_CQ + h * 64:O_CQ + (h + 1) * 64].rearrange("(t p) c -> p t c", p=128))
            load(kb, sk, sk.t[:], dr['proj'][:, O_CK + h * 64:O_CK + (h + 1) * 64].rearrange("(t p) c -> p t c", p=128))
            load(kb, sv, sv.t[:], dr['proj'][:, O_CV + h * 64:O_CV + (h + 1) * 64].rearrange("(r p) c -> p r c", p=64))
            kb.op('dve', [sq.b], [sqb.b], lambda e: e.tensor_copy(out=sqb.t[:], in_=sq.t[:]))
            kb.op('pool', [sk.b], [skb.b], lambda e: e.tensor_copy(out=skb.t[:], in_=sk.t[:]))
            kb.op('act', [sv.b], [vr.b], lambda e: e.copy(out=vr.t[:, :, 0:64], in_=sv.t[:]))
            for (src, dst) in ((sqb, qT), (skb, kT)):
                for t0 in range(0, NT, 4):
                    n = min(4, NT - t0); pt = ptr.next()
                    for j in range(n):
                        kb.op('pe', [src.b, ident.b], [pt.b], lambda e: e.transpose(out=pt.t[:, j, :], in_=src.t[:, t0 + j, :], identity=ident.t[:]))
                    kb.op('act', [pt.b], [dst.b], lambda e: e.copy(out=dst.t[:, t0 * 128:(t0 + n) * 128].rearrange("p (j c) -> p j c", j=n), in_=pt.t[:, 0:n, :]))
            O = None
            for r in range(NAT_ROWS if CUT else 64):
                rs = min(max(r - 4, 0), 56); dr0 = rs - r + 7
                if r % 7 == 0:
                    O = Or.next(); rbase = r
                S = Sr.next(); Sc = Scr.next(); Sb = Sbr.next(); P = Pr.next(); Pc = Pcr.next()
                qs_ = qT.t[:, r * 64:(r + 1) * 64]
                for i in range(8):
                    kb.op('pe', [kT.b, qT.b], [S.b], lambda e: e.matmul(S.t[:, i, :], lhsT=kT.t[:, (rs + i) * 64:(rs + i + 1) * 64], rhs=qs_, start=True, stop=True))
                for i in range(4):
                    kb.op('pe', [kT.b, qT.b], [Sc.b], lambda e: e.matmul(Sc.t[:, i, 0:64], lhsT=kT.t[:, L + i * 64:L + (i + 1) * 64], rhs=qs_, start=True, stop=True))
                kb.op('dve', [S.b, bias.b], [Sb.b], lambda e: e.scalar_tensor_tensor(out=Sb.t[:], in0=S.t[:], scalar=0.125, in1=bias.t[:, dr0:dr0 + 8, :], op0=ALU.mult, op1=ALU.add))
                kb.op('act', [Sb.b], [P.b], lambda e: e.activation(out=P.t[:], in_=Sb.t[:], func=AF.Exp))
                kb.op('act', [Sc.b], [Pc.b], lambda e: e.activation(out=Pc.t[:, :, 0:64], in_=Sc.t[:, :, 0:64], func=AF.Exp, scale=0.125))
                oi = r - rbase
                for i in range(12):
                    lhs = P.t[:, i, :] if i < 8 else Pc.t[:, i - 8, 0:64]
                    vrow = rs + i if i < 8 else 64 + (i - 8)
                    kb.op('pe', [P.b, Pc.b, vr.b], [O.b], lambda e: e.matmul(O.t[:, oi, 0:65], lhsT=lhs, rhs=vr.t[:, vrow, 0:65], start=(i == 0), stop=(i == 11)))
                if oi == 6 or r == (NAT_ROWS if CUT else 64) - 1:
                    n = oi + 1
                    kb.op('dve', [O.b], [rec.b], lambda e: e.reciprocal(out=rec.t[:, 0:n], in_=O.t[:, 0:n, 64]))
                    kb.op('dve', [O.b, rec.b], [yo.b], lambda e: e.tensor_tensor(out=yo.t[:, rbase:rbase + n, :], in0=O.t[:, 0:n, 0:64], in1=rec.t[:, 0:n].unsqueeze(2).to_broadcast([64, n, 64]), op=ALU.mult))
            nrows = 64
            if ctx_out:
                nrows = 68
                Sc = Scr.next(); Pc = Pcr.next(); O = Or.next()
                for i in range(4):
                    kb.op('pe', [kT.b, qT.b], [Sc.b], lambda e: e.matmul(Sc.t[:, i, :], lhsT=kT.t[:, L + i * 64:L + (i + 1) * 64], rhs=qT.t[:, L:T], start=True, stop=True))
                kb.op('act', [Sc.b], [Pc.b], lambda e: e.activation(out=Pc.t[:], in_=Sc.t[:], func=AF.Exp, scale=0.125))
                for qs in range(4):
                    for i in range(4):
                        kb.op('pe', [Pc.b, vr.b], [O.b], lambda e: e.matmul(O.t[:, qs, 0:65], lhsT=Pc.t[:, i, qs * 64:(qs + 1) * 64], rhs=vr.t[:, 64 + i, 0:65], start=(i == 0), stop=(i == 3)))
                kb.op('dve', [O.b], [rec.b], lambda e: e.reciprocal(out=rec.t[:, 0:4], in_=O.t[:, 0:4, 64]))
                kb.op('dve', [O.b, rec.b], [yo.b], lambda e: e.tensor_tensor(out=yo.t[:, 64:68, :], in0=O.t[:, 0:4, 0:64], in1=rec.t[:, 0:4].unsqueeze(2).to_broadcast([64, 4, 64]), op=ALU.mult))
            store(kb, yo, dr['ybr'][0:nrows * 64, 1024 + h * 64:1024 + (h + 1) * 64].rearrange("(r p) c -> p r c", p=64), yo.t[:, 0:nrows, :], eng='sp')


def phase_gdn_prep(kb, dr, l):
    with Phase(kb) as ph:
        cw = ph.sb([64, 5, 1536], F32, dma=True)
        load(kb, cw, cw.t[:], dr['gdn_conv'][l].partition_broadcast(64))
        dtb = ph.sb([64, 8], F32, dma=True); nA = ph.sb([64, 8], F32, dma=True)
        load(kb, dtb, dtb.t[:], dr['gdn_dt_bias'][l].rearrange("a b -> (a b)").partition_broadcast(64))
        load(kb, nA, nA.t[:], dr['gdn_a_log'][l].rearrange("a b -> (a b)").partition_broadcast(64))
        kb.op('act', [nA.b], [nA.b], lambda e: e.activation(out=nA.t[:], in_=nA.t[:], func=AF.Exp))
        kb.op('dve', [nA.b], [nA.b], lambda e: e.tensor_scalar(out=nA.t[:], in0=nA.t[:], scalar1=-1.0, scalar2=None, op0=ALU.mult))
        ba = ph.sb([64, 68, 16], F32, dma=True); bg = ph.sb([64, 68, 16], F32, dma=True)
        load(kb, ba, ba.t[:], dr['proj'][:, O_BA:O_BA + 16].rearrange("(c p) k -> p c k", p=64))
        kb.op('act', [ba.b], [bg.b], lambda e: e.activation(out=bg.t[:, :, 0:8], in_=ba.t[:, :, 0:8], func=AF.Sigmoid))
        kb.op('dve', [ba.b, dtb.b], [ba.b], lambda e: e.tensor_tensor(out=ba.t[:, :, 8:16], in0=ba.t[:, :, 8:16], in1=dtb.t[:].unsqueeze(1).to_broadcast([64, 68, 8]), op=ALU.add))
        kb.op('act', [ba.b], [ba.b], lambda e: e.activation(out=ba.t[:, :, 8:16], in_=ba.t[:, :, 8:16], func=AF.Exp))
        kb.op('act', [ba.b], [ba.b], lambda e: e.activation(out=ba.t[:, :, 8:16], in_=ba.t[:, :, 8:16], func=AF.Ln, bias=1.0))
        kb.op('dve', [ba.b, nA.b], [bg.b], lambda e: e.tensor_tensor(out=bg.t[:, :, 8:16], in0=ba.t[:, :, 8:16], in1=nA.t[:].unsqueeze(1).to_broadcast([64, 68, 8]), op=ALU.mult))
        store(kb, bg, dr['gbg'].rearrange("(c p) k -> p c k", p=64), bg.t[:], eng='sp')
        xsr = ph.ring(2, [64, 5, 1536], F32, dma=True)
        acc = ph.sb([64, 1536], F32); tmpr = ph.ring(2, [64, 1536], F32); sact = ph.sb([64, 1536], F32)
        sq = ph.sb([64, 1024], F32); n8 = ph.sb([64, 8], F32)
        outr = ph.ring(2, [64, 1536], F32, dma=True)
        for c in range(68):
            seg_lo, seg_hi = (0, L) if c < 64 else (L, T)
            xs = xsr.next(); o = outr.next()
            for j in range(5):
                r0 = c * 64 + j - 2
                lo = max(r0, seg_lo); hi = min(r0 + 64, seg_hi)
                if lo > r0 or hi < r0 + 64:
                    kb.op('pool', [], [xs.b], lambda e: e.memset(xs.t[:, j, :], 0.0))
                load(kb, xs, xs.t[lo - r0:hi - r0, j, :], dr['proj'][lo:hi, 0:1536])
            kb.op('dve', [xs.b, cw.b], [acc.b], lambda e: e.tensor_tensor(out=acc.t[:], in0=xs.t[:, 0, :], in1=cw.t[:, 0, :], op=ALU.mult))
            for j in range(1, 5):
                tm = tmpr.next()
                kb.op('dve', [xs.b, cw.b], [tm.b], lambda e: e.tensor_tensor(out=tm.t[:], in0=xs.t[:, j, :], in1=cw.t[:, j, :], op=ALU.mult))
                kb.op('dve', [acc.b, tm.b], [acc.b], lambda e: e.tensor_tensor(out=acc.t[:], in0=acc.t[:], in1=tm.t[:], op=ALU.add))
            kb.op('act', [acc.b], [sact.b], lambda e: e.activation(out=sact.t[:], in_=acc.t[:], func=AF.Silu))
            kb.op('dve', [sact.b], [sq.b], lambda e: e.tensor_tensor(out=sq.t[:], in0=sact.t[:, 0:1024], in1=sact.t[:, 0:1024], op=ALU.mult))
            kb.op('dve', [sq.b], [n8.b], lambda e: e.reduce_sum(out=n8.t[:], in_=sq.t[:].rearrange("p (h d) -> p h d", h=8), axis=AX.X))
            kb.op('dve', [n8.b], [n8.b], lambda e: e.tensor_scalar(out=n8.t[:], in0=n8.t[:], scalar1=EPS, scalar2=None, op0=ALU.add))
            kb.op('act', [n8.b], [n8.b], lambda e: e.sqrt(out=n8.t[:], in_=n8.t[:]))
            kb.op('dve', [n8.b], [n8.b], lambda e: e.reciprocal(out=n8.t[:], in_=n8.t[:]))
            kb.op('dve', [n8.b], [n8.b], lambda e: e.tensor_scalar(out=n8.t[:, 0:4], in0=n8.t[:, 0:4], scalar1=128.0 ** -0.5, scalar2=None, op0=ALU.mult))
            kb.op('dve', [sact.b, n8.b], [o.b], lambda e: e.tensor_tensor(out=o.t[:, 0:1024].rearrange("p (h d) -> p h d", h=8), in0=sact.t[:, 0:1024].rearrange("p (h d) -> p h d", h=8),
                  in1=n8.t[:].unsqueeze(2).to_broadcast([64, 8, 128]), op=ALU.mult))
            kb.op('act', [sact.b], [o.b], lambda e: e.copy(out=o.t[:, 1024:1536], in_=sact.t[:, 1024:1536]))
            store(kb, o, dr['gqkv'][c * 64:(c + 1) * 64, :], o.t[:], eng='sp')


def phase_gdn(kb, dr, l, ident):
    NCH = 68
    with Phase(kb) as ph:
        id64 = ident.t[0:64, 0:64]
        gcn = ph.sb([64, 5, 64], F32, dma=True)
        load(kb, gcn, gcn.t[:], dr['gconst'])
        Lb16 = ph.sb([64, 2, 64], BF16); ones16 = ph.sb([64, 128], BF16)
        kb.op('dve', [gcn.b], [Lb16.b], lambda e: e.tensor_copy(out=Lb16.t[:], in_=gcn.t[:, 0:2, :]))
        kb.op('pool', [], [ones16.b], lambda e: e.memset(ones16.t[:], 1.0))
        gg = ph.sb([64, 128], F32, dma=True)
        load(kb, gg, gg.t[:], dr['gdn_norm_g'][l].partition_broadcast(64))
        bgall = ph.sb([64, NCH, 16], F32, dma=True)
        load(kb, bgall, bgall.t[:], dr['gbg'].rearrange("(c p) k -> p c k", p=64))
        banks = ph.ring(7, [128, 512], F32, psum=True)
        bt = ph.ps([128, 8, 64], BF16)
        v3 = lambda bank, np_, a, b: bank.t[0:np_, 0:a * b].rearrange("p (a b) -> p a b", b=b)
        ghi = ph.sb([64, NCH, 8], BF16); glo = ph.sb([64, NCH, 8], BF16); gtmp = ph.sb([64, NCH, 8], F32)
        gc = ph.sb([64, NCH, 8], F32); glB = ph.sb([64, NCH, 8], F32); Sdec = ph.sb([128, NCH, 8], F32)
        egc = ph.sb([64, NCH, 8], F32); kds = ph.sb([64, NCH, 8], F32); bgs = ph.sb([64, NCH, 8], F32); nbeta = ph.sb([64, NCH, 8], F32)
        gv = bgall.t[:, :, 8:16]
        kb.op('dve', [bgall.b], [ghi.b], lambda e: e.tensor_copy(out=ghi.t[:], in_=gv))
        kb.op('dve', [ghi.b], [gtmp.b], lambda e: e.tensor_copy(out=gtmp.t[:], in_=ghi.t[:]))
        kb.op('dve', [bgall.b, gtmp.b], [glo.b], lambda e: e.tensor_tensor(out=glo.t[:], in0=gv, in1=gtmp.t[:], op=ALU.subtract))
        for half in range(2):
            cs = slice(half * 34, (half + 1) * 34)
            outs = []
            for (lhs, np_) in ((Lb16.t[:, 0, :], 64), (Lb16.t[:, 1, :], 64), (ones16.t[:, 0:64], 64), (ones16.t[:], 128)):
                pb = banks.next(); outs.append(pb)
                kb.op('pe', [Lb16.b, ones16.b, ghi.b], [pb.b], lambda e: e.matmul(v3(pb, np_, 34, 8), lhsT=lhs, rhs=ghi.t[:, cs, :], start=True, stop=False))
                kb.op('pe', [Lb16.b, ones16.b, glo.b], [pb.b], lambda e: e.matmul(v3(pb, np_, 34, 8), lhsT=lhs, rhs=glo.t[:, cs, :], start=False, stop=True))
            kb.op('act', [outs[0].b], [gc.b], lambda e: e.copy(out=gc.t[:, cs, 0:4], in_=v3(outs[0], 64, 34, 8)[:, :, 0:4]))
            kb.op('act', [outs[1].b], [gc.b], lambda e: e.copy(out=gc.t[:, cs, 4:8], in_=v3(outs[1], 64, 34, 8)[:, :, 4:8]))
            kb.op('dve', [outs[2].b], [glB.b], lambda e: e.tensor_copy(out=glB.t[:, cs, :], in_=v3(outs[2], 64, 34, 8)))
            kb.op('act', [outs[3].b], [Sdec.b], lambda e: e.activation(out=Sdec.t[:, cs, :], in_=v3(outs[3], 128, 34, 8), func=AF.Exp))
        kb.op('act', [gc.b], [egc.b], lambda e: e.activation(out=egc.t[:], in_=gc.t[:], func=AF.Exp))
        kb.op('dve', [glB.b, gc.b], [kds.b], lambda e: e.tensor_tensor(out=kds.t[:], in0=glB.t[:], in1=gc.t[:], op=ALU.subtract))
        kb.op('act', [kds.b], [kds.b], lambda e: e.activation(out=kds.t[:], in_=kds.t[:], func=AF.Exp))
        kb.op('dve', [bgall.b, egc.b], [bgs.b], lambda e: e.tensor_tensor(out=bgs.t[:], in0=bgall.t[:, :, 0:8], in1=egc.t[:], op=ALU.mult))
        kb.op('dve', [bgall.b], [nbeta.b], lambda e: e.tensor_scalar(out=nbeta.t[:], in0=bgall.t[:, :, 0:8], scalar1=-1.0, scalar2=None, op0=ALU.mult))
        k_tok = ph.sb([64, NCH, 128], BF16); v_tok = ph.sb([64, NCH, 128], BF16)
        kT = ph.sb([128, NCH * 64], BF16); qT = ph.sb([128, NCH * 64], BF16)
        stg = ph.sb([64, 17, 128], F32, dma=True); qst = ph.sb([64, 17, 128], BF16)
        TT = ph.sb([64, NCH, 64], BF16); P2 = ph.sb([64, NCH, 64], BF16); negwT = ph.sb([128, NCH * 64], BF16)
        o_all = ph.sb([64, NCH, 128], F32, dma=True)
        gch = ph.sb([64, 8], BF16); gchf = ph.sb([64, 8], F32); gcl = ph.sb([64, 8], BF16)
        gcrh = ph.sb([64, 8, 64], BF16); gcrl = ph.sb([64, 8, 64], BF16)
        Xs = ph.sb([64, 8, 64], F32); Xp = ph.sb([64, 8, 64], F32); Xn = ph.sb([64, 8, 64], F32)
        xr = ph.ring(3, [64, 8, 64], BF16); yr_ = ph.ring(3, [64, 8, 64], BF16); rr = ph.ring(3, [64, 8, 64], BF16)
        kbg = ph.sb([64, 8, 128], BF16)
        S = ph.sb([128, 128], F32); Sb = ph.sb([128, 128], BF16)
        vbr = ph.ring(2, [64, 128], BF16); kdr = ph.ring(2, [64, 128], BF16); vnr = ph.ring(2, [64, 128], BF16)
        t1r = ph.ring(2, [64, 128], F32); t2r = ph.ring(2, [64, 128], F32)
        rs17 = ph.sb([64, 17], F32); sq17 = Xs
        print('gdn sbuf bytes remaining', kb.nc.sbuf_bytes_remaining)
        for h in range(1 if CUT else 4):
            for (col0, kind) in ((512, 'k'), (1024, 'v'), (0, 'q')):
                for pc in range(4):
                    c0 = pc * 17
                    load(kb, stg, stg.t[:], dr['gqkv'][c0 * 64:(c0 + 17) * 64, col0 + h * 128:col0 + (h + 1) * 128].rearrange("(c p) d -> p c d", p=64))
                    if kind == 'k':
                        kb.op('dve', [stg.b], [k_tok.b], lambda e: e.tensor_copy(out=k_tok.t[:, c0:c0 + 17, :], in_=stg.t[:]))
                    elif kind == 'v':
                        kb.op('dve', [stg.b], [v_tok.b], lambda e: e.tensor_copy(out=v_tok.t[:, c0:c0 + 17, :], in_=stg.t[:]))
                    else:
                        kb.op('dve', [stg.b], [qst.b], lambda e: e.tensor_copy(out=qst.t[:], in_=stg.t[:]))
                    if kind in ('k', 'q'):
                        src = k_tok if kind == 'k' else qst
                        dst = kT if kind == 'k' else qT
                        for g0 in range(0, 17, 8):
                            n = min(8, 17 - g0)
                            for j in range(n):
                                cc = (c0 + g0 + j) if kind == 'k' else (g0 + j)
                                kb.op('pe', [src.b, ident.b], [bt.b], lambda e: e.transpose(out=bt.t[:, j, :], in_=src.t[:, cc, :], identity=id64))
                            kb.op('act', [bt.b], [dst.b], lambda e: e.copy(out=dst.t[:, (c0 + g0) * 64:(c0 + g0 + n) * 64].rearrange("p (a b) -> p a b", b=64), in_=bt.t[:, 0:n, :]))
            for d in range(2):
                hd = d * 4 + h
                LmT = gcn.t[:, d, :]; Sm = gcn.t[:, 2 + d, :]
                for c0 in range(0, (16 if CUT == 'x' else NCH), 8):
                    n = min(8, NCH - c0)
                    csl = slice(c0, c0 + n)
                    bc = lambda ap2: ap2.unsqueeze(2).to_broadcast([64, n, 64])
                    bm = lambda ap2: ap2.unsqueeze(1).to_broadcast([64, n, 64])
                    kb.op('dve', [gc.b], [gch.b], lambda e: e.tensor_copy(out=gch.t[:, 0:n], in_=gc.t[:, csl, hd]))
                    kb.op('dve', [gch.b], [gchf.b], lambda e: e.tensor_copy(out=gchf.t[:, 0:n], in_=gch.t[:, 0:n]))
                    kb.op('dve', [gc.b, gchf.b], [gcl.b], lambda e: e.tensor_tensor(out=gcl.t[:, 0:n], in0=gc.t[:, csl, hd], in1=gchf.t[:, 0:n], op=ALU.subtract))
                    kb.op('dve', [gch.b], [gcrh.b], lambda e: e.tensor_copy(out=gcrh.t[:, 0:n, :], in_=bc(gch.t[:, 0:n])))
                    kb.op('dve', [gcl.b], [gcrl.b], lambda e: e.tensor_copy(out=gcrl.t[:, 0:n, :], in_=bc(gcl.t[:, 0:n])))
                    pG = banks.next(); pKK = banks.next(); pQK = banks.next()
                    for j in range(n):
                        c = c0 + j; cc = slice(c * 64, (c + 1) * 64)
                        kb.op('pe', [gcrh.b, ident.b], [pG.b], lambda e: e.matmul(v3(pG, 64, n, 64)[:, j, :], lhsT=gcrh.t[:, j, :], rhs=id64, start=True, stop=False))
                        kb.op('pe', [gcrl.b, ident.b], [pG.b], lambda e: e.matmul(v3(pG, 64, n, 64)[:, j, :], lhsT=gcrl.t[:, j, :], rhs=id64, start=False, stop=True))
                        kb.op('pe', [kT.b], [pKK.b], lambda e: e.matmul(v3(pKK, 64, n, 64)[:, j, :], lhsT=kT.t[:, cc], rhs=kT.t[:, cc], start=True, stop=True))
                        kb.op('pe', [kT.b, qT.b], [pQK.b], lambda e: e.matmul(v3(pQK, 64, n, 64)[:, j, :], lhsT=kT.t[:, cc], rhs=qT.t[:, cc], start=True, stop=True))
                    kb.op('dve', [pG.b, gc.b], [Xs.b], lambda e: e.tensor_tensor(out=Xs.t[:, 0:n, :], in0=v3(pG, 64, n, 64), in1=bc(gc.t[:, csl, hd]), op=ALU.subtract))
                    kb.op('dve', [Xs.b], [Xp.b], lambda e: e.tensor_scalar(out=Xp.t[:, 0:n, :], in0=Xs.t[:, 0:n, :], scalar1=0.0, scalar2=None, op0=ALU.max))
                    kb.op('dve', [Xs.b], [Xn.b], lambda e: e.tensor_scalar(out=Xn.t[:, 0:n, :], in0=Xs.t[:, 0:n, :], scalar1=0.0, scalar2=None, op0=ALU.min))
                    kb.op('act', [Xp.b], [Xp.b], lambda e: e.activation(out=Xp.t[:, 0:n, :], in_=Xp.t[:, 0:n, :], func=AF.Exp, scale=-1.0))
                    kb.op('act', [Xn.b], [Xn.b], lambda e: e.activation(out=Xn.t[:, 0:n, :], in_=Xn.t[:, 0:n, :], func=AF.Exp))
                    kb.op('dve', [Xp.b, gcn.b], [Xp.b], lambda e: e.tensor_tensor(out=Xp.t[:, 0:n, :], in0=Xp.t[:, 0:n, :], in1=bm(Sm), op=ALU.mult))
                    kb.op('dve', [Xp.b, nbeta.b], [Xp.b], lambda e: e.tensor_tensor(out=Xp.t[:, 0:n, :], in0=Xp.t[:, 0:n, :], in1=bc(nbeta.t[:, csl, hd]), op=ALU.mult))
                    X = xr.next()
                    kb.op('dve', [pKK.b, Xp.b], [X.b], lambda e: e.tensor_tensor(out=X.t[:, 0:n, :], in0=v3(pKK, 64, n, 64), in1=Xp.t[:, 0:n, :], op=ALU.mult))
                    kb.op('dve', [Xn.b, gcn.b], [Xn.b], lambda e: e.tensor_tensor(out=Xn.t[:, 0:n, :], in0=Xn.t[:, 0:n, :], in1=bm(LmT), op=ALU.mult))
                    kb.op('dve', [pQK.b, Xn.b], [P2.b], lambda e: e.tensor_tensor(out=P2.t[:, csl, :], in0=v3(pQK, 64, n, 64), in1=Xn.t[:, 0:n, :], op=ALU.mult))
                    for j in range(n):
                        kb.op('pe', [X.b, ident.b], [bt.b], lambda e: e.transpose(out=bt.t[0:64, j, :], in_=X.t[:, j, :], identity=id64))
                    Y = yr_.next(); R = rr.next()
                    kb.op('act', [bt.b], [Y.b], lambda e: e.copy(out=Y.t[:, 0:n, :], in_=bt.t[0:64, 0:n, :]))
                    kb.op('dve', [Y.b, ident.b], [R.b], lambda e: e.tensor_tensor(out=R.t[:, 0:n, :], in0=Y.t[:, 0:n, :], in1=bm(id64), op=ALU.add))
                    for m in range(5):
                        pX = banks.next(); Xn_ = xr.next()
                        for j in range(n):
                            kb.op('pe', [X.b, Y.b], [pX.b], lambda e: e.matmul(v3(pX, 64, n, 64)[:, j, :], lhsT=Y.t[:, j, :], rhs=X.t[:, j, :], start=True, stop=True))
                        kb.op('act', [pX.b], [Xn_.b], lambda e: e.copy(out=Xn_.t[:, 0:n, :], in_=v3(pX, 64, n, 64)))
                        if m < 4:
                            pY = banks.next(); Yn_ = yr_.next()
                            for j in range(n):
                                kb.op('pe', [X.b, Y.b], [pY.b], lambda e: e.matmul(v3(pY, 64, n, 64)[:, j, :], lhsT=X.t[:, j, :], rhs=Y.t[:, j, :], start=True, stop=True))
                            kb.op('dve', [pY.b], [Yn_.b], lambda e: e.tensor_copy(out=Yn_.t[:, 0:n, :], in_=v3(pY, 64, n, 64)))
                        pR = banks.next()
                        for j in range(n):
                            kb.op('pe', [R.b, ident.b], [pR.b], lambda e: e.matmul(v3(pR, 64, n, 64)[:, j, :], lhsT=id64, rhs=R.t[:, j, :], start=True, stop=False))
                            kb.op('pe', [R.b, Xn_.b], [pR.b], lambda e: e.matmul(v3(pR, 64, n, 64)[:, j, :], lhsT=Xn_.t[:, j, :], rhs=R.t[:, j, :], start=False, stop=True))
                        if m < 4:
                            Rn_ = rr.next()
                            kb.op('act', [pR.b], [Rn_.b], lambda e: e.copy(out=Rn_.t[:, 0:n, :], in_=v3(pR, 64, n, 64)))
                            R = Rn_; Y = Yn_
                        else:
                            kb.op('act', [pR.b], [TT.b], lambda e: e.copy(out=TT.t[:, csl, :], in_=v3(pR, 64, n, 64)))
                        X = Xn_
                    kb.op('dve', [k_tok.b, bgs.b], [kbg.b], lambda e: e.tensor_tensor(out=kbg.t[:, 0:n, :], in0=k_tok.t[:, csl, :], in1=bgs.t[:, csl, hd].unsqueeze(2).to_broadcast([64, n, 128]), op=ALU.mult))
                    pW = banks.next()
                    for j in range(n):
                        kb.op('pe', [kbg.b, TT.b], [pW.b], lambda e: e.matmul(v3(pW, 128, n, 64)[:, j, :], lhsT=kbg.t[:, j, :], rhs=TT.t[:, c0 + j, :], start=True, stop=True))
                    kb.op('act', [pW.b], [negwT.b], lambda e: e.mul(out=negwT.t[:, c0 * 64:(c0 + n) * 64].rearrange("p (a b) -> p a b", b=64), in_=v3(pW, 128, n, 64), mul=-1.0))
                kb.op('pool', [], [S.b], lambda e: e.memset(S.t[:], 0.0))
                kb.op('pool', [], [Sb.b], lambda e: e.memset(Sb.t[:], 0.0))
                order = ([64, 65, 66, 67] + list(range(64))) if d == 0 else ([67, 66, 65, 64] + list(range(63, -1, -1)))
                if CUT == 'x':
                    order = list(range(16)) if d == 0 else list(range(15, -1, -1))
                for c in order:
                    cc = slice(c * 64, (c + 1) * 64)
                    vb = vbr.next(); kd = kdr.next(); vn = vnr.next(); t1 = t1r.next()
                    kb.op('dve', [v_tok.b, bgall.b], [vb.b], lambda e: e.tensor_scalar(out=vb.t[:], in0=v_tok.t[:, c, :], scalar1=bgall.t[:, c, hd:hd + 1], scalar2=None, op0=ALU.mult))
                    kb.op('dve', [k_tok.b, kds.b], [kd.b], lambda e: e.tensor_scalar(out=kd.t[:], in0=k_tok.t[:, c, :], scalar1=kds.t[:, c, hd:hd + 1], scalar2=None, op0=ALU.mult))
                    pV = banks.next()
                    kb.op('pe', [TT.b, vb.b], [pV.b], lambda e: e.matmul(pV.t[0:64, 0:128], lhsT=TT.t[:, c, :], rhs=vb.t[:], start=True, stop=False))
                    kb.op('pe', [negwT.b, Sb.b], [pV.b], lambda e: e.matmul(pV.t[0:64, 0:128], lhsT=negwT.t[:, cc], rhs=Sb.t[:], start=False, stop=True))
                    kb.op('act', [pV.b], [vn.b], lambda e: e.copy(out=vn.t[:], in_=pV.t[0:64, 0:128]))
                    pO1 = banks.next(); pO2 = banks.next(); pS = banks.next()
                    kb.op('pe', [qT.b, Sb.b], [pO1.b], lambda e: e.matmul(pO1.t[0:64, 0:128], lhsT=qT.t[:, cc], rhs=Sb.t[:], start=True, stop=True))
                    kb.op('pe', [P2.b, vn.b], [pO2.b], lambda e: e.matmul(pO2.t[0:64, 0:128], lhsT=P2.t[:, c, :], rhs=vn.t[:], start=True, stop=True))
                    kb.op('pe', [kd.b, vn.b], [pS.b], lambda e: e.matmul(pS.t[:, 0:128], lhsT=kd.t[:], rhs=vn.t[:], start=True, stop=True))
                    kb.op('dve', [S.b, Sdec.b, pS.b], [S.b], lambda e: e.scalar_tensor_tensor(out=S.t[:], in0=S.t[:], scalar=Sdec.t[:, c, hd:hd + 1], in1=pS.t[:, 0:128], op0=ALU.mult, op1=ALU.add))
                    kb.op('act', [S.b], [Sb.b], lambda e: e.copy(out=Sb.t[:], in_=S.t[:]))
                    kb.op('act', [pO1.b, egc.b], [t1.b], lambda e: e.activation(out=t1.t[:], in_=pO1.t[0:64, 0:128], func=AF.Copy, scale=egc.t[:, c, hd:hd + 1]))
                    if d == 0:
                        kb.op('dve', [t1.b, pO2.b], [o_all.b], lambda e: e.tensor_tensor(out=o_all.t[:, c, :], in0=t1.t[:], in1=pO2.t[0:64, 0:128], op=ALU.add))
                    else:
                        t2 = t2r.next()
                        kb.op('dve', [t1.b, pO2.b], [t2.b], lambda e: e.tensor_tensor(out=t2.t[:], in0=t1.t[:], in1=pO2.t[0:64, 0:128], op=ALU.add))
                        kb.op('dve', [t2.b, o_all.b], [o_all.b], lambda e: e.tensor_tensor(out=o_all.t[:, c, :], in0=o_all.t[:, c, :], in1=t2.t[:], op=ALU.add))
            for pc in range(1 if CUT == 'x' else 4):
                c0 = pc * 17; csl = slice(c0, c0 + 17)
                load(kb, stg, stg.t[:], dr['proj'][c0 * 64:(c0 + 17) * 64, O_AZ + h * 128:O_AZ + (h + 1) * 128].rearrange("(c p) d -> p c d", p=64))
                kb.op('act', [stg.b], [stg.b], lambda e: e.activation(out=stg.t[:], in_=stg.t[:], func=AF.Silu))
                for g0 in range(0, 17, 8):
                    n = min(8, 17 - g0); gs_ = slice(c0 + g0, c0 + g0 + n)
                    sqv = Xs.t[:, 0:n, :]
                    kb.op('dve', [o_all.b], [Xs.b], lambda e: e.tensor_tensor(out=sqv, in0=o_all.t[:, gs_, 0:64], in1=o_all.t[:, gs_, 0:64], op=ALU.mult))
                    kb.op('dve', [o_all.b], [Xp.b], lambda e: e.tensor_tensor(out=Xp.t[:, 0:n, :], in0=o_all.t[:, gs_, 64:128], in1=o_all.t[:, gs_, 64:128], op=ALU.mult))
                    kb.op('dve', [Xs.b, Xp.b], [Xs.b], lambda e: e.tensor_tensor(out=sqv, in0=sqv, in1=Xp.t[:, 0:n, :], op=ALU.add))
                    kb.op('dve', [Xs.b], [rs17.b], lambda e: e.reduce_sum(out=rs17.t[:, g0:g0 + n], in_=sqv, axis=AX.X))
                kb.op('dve', [rs17.b], [rs17.b], lambda e: e.tensor_scalar(out=rs17.t[:], in0=rs17.t[:], scalar1=1.0 / 128, scalar2=EPS, op0=ALU.mult, op1=ALU.add))
                kb.op('act', [rs17.b], [rs17.b], lambda e: e.sqrt(out=rs17.t[:], in_=rs17.t[:]))
                kb.op('dve', [rs17.b], [rs17.b], lambda e: e.reciprocal(out=rs17.t[:], in_=rs17.t[:]))
                kb.op('dve', [o_all.b, rs17.b], [o_all.b], lambda e: e.tensor_tensor(out=o_all.t[:, csl, :], in0=o_all.t[:, csl, :], in1=rs17.t[:].unsqueeze(2).to_broadcast([64, 17, 128]), op=ALU.mult))
                kb.op('dve', [o_all.b, gg.b], [o_all.b], lambda e: e.tensor_tensor(out=o_all.t[:, csl, :], in0=o_all.t[:, csl, :], in1=gg.t[:].unsqueeze(1).to_broadcast([64, 17, 128]), op=ALU.mult))
                kb.op('dve', [o_all.b, stg.b], [o_all.b], lambda e: e.tensor_tensor(out=o_all.t[:, csl, :], in0=o_all.t[:, csl, :], in1=stg.t[:], op=ALU.mult))
                store(kb, o_all, dr['ybr'][c0 * 64:(c0 + 17) * 64, h * 128:(h + 1) * 128].rearrange("(c p) d -> p c d", p=64), o_all.t[:, csl, :], eng='sp')


def load_bf16_weight(kb, ph, dst, dst_chunks, src_chunk_aps, stg):
    for i, ap in enumerate(src_chunk_aps):
        s_ = stg.next()
        n = ap.shape[-1]
        load(kb, s_, s_.t[:, :n], ap)
        kb.op('pool', [s_.b], [dst.b], lambda e: e.tensor_copy(out=dst.t[:, i, :n], in_=s_.t[:, :n]))


def phase_merge(kb, dr, l, ident, ctx_out):
    ntile = NT if ctx_out else 32
    if CUT:
        ntile = 1
    with Phase(kb) as ph:
        stg = ph.ring(2, [128, 1024], F32, dma=True)
        wup = ph.sb([128, 12, 1024], BF16); wout = ph.sb([128, 8, 1024], BF16)
        load_bf16_weight(kb, ph, wup, 12, [dr['w_up'][l, i // 4, (i % 4) * 128:(i % 4 + 1) * 128, :] for i in range(12)], stg)
        load_bf16_weight(kb, ph, wout, 8, [dr['w_out'][l, i * 128:(i + 1) * 128, :] for i in range(8)], stg)
        gts = []
        for r in range(2):
            g = ph.sb([128, D], F32, dma=True)
            load(kb, g, g.t[:], dr['mod'][l, r, 2 * D:3 * D].partition_broadcast(128))
            gts.append(g)
        yr = ph.ring(2, [128, 1536], F32, dma=True); ybr_ = ph.ring(2, [128, 1536], BF16)
        gr = ph.ring(2, [128, 3, 1024], F32, dma=True)
        xr = ph.ring(2, [128, D], F32, dma=True)
        yT = ph.sb([128, 12, 128], BF16); mT = ph.sb([128, 8, 128], BF16)
        t0 = ph.sb([128, 512], F32); t1 = ph.sb([128, 512], F32); mb = ph.sb([128, 1024], BF16)
        ptY = ph.ps([128, 12, 128], BF16); ptM = ph.ps([128, 8, 128], BF16)
        upr = ph.ring(3, [128, 512], F32, psum=True); o2r = ph.ring(2, [128, 512], F32, psum=True)
        for t in range(ntile):
            rows = slice(t * 128, (t + 1) * 128)
            y = yr.next(); yb = ybr_.next(); g = gr.next(); xt = xr.next()
            load(kb, y, y.t[:], dr['ybr'][rows, :])
            load(kb, g, g.t[:], dr['proj'][rows, O_G:O_G + 3072].rearrange("p (n c) -> p n c", n=3))
            load(kb, xt, xt.t[:], x_src(dr, l, t))
            kb.op('dve', [y.b], [yb.b], lambda e: e.tensor_copy(out=yb.t[:], in_=y.t[:]))
            kb.op('act', [g.b], [g.b], lambda e: e.activation(out=g.t[:], in_=g.t[:], func=AF.Sigmoid))
            for k in range(12):
                kb.op('pe', [yb.b, ident.b], [ptY.b], lambda e: e.transpose(out=ptY.t[:, k, :], in_=yb.t[:, k * 128:(k + 1) * 128], identity=ident.t[:]))
            kb.op('act', [ptY.b], [yT.b], lambda e: e.copy(out=yT.t[:], in_=ptY.t[:]))
            for half in range(2):
                cs = slice(half * 512, (half + 1) * 512)
                ups = []
                for n in range(3):
                    up = upr.next(); ups.append(up)
                    for kc in range(4):
                        kb.op('pe', [yT.b, wup.b], [up.b], lambda e: e.matmul(up.t[:], lhsT=yT.t[:, n * 4 + kc, :], rhs=wup.t[:, n * 4 + kc, cs], start=(kc == 0), stop=(kc == 3)))
                kb.op('dve', [ups[0].b, g.b], [t0.b], lambda e: e.tensor_tensor(out=t0.t[:], in0=ups[0].t[:], in1=g.t[:, 0, cs], op=ALU.mult))
                kb.op('dve', [ups[1].b, g.b], [t1.b], lambda e: e.tensor_tensor(out=t1.t[:], in0=ups[1].t[:], in1=g.t[:, 1, cs], op=ALU.mult))
                kb.op('dve', [t0.b, t1.b], [t0.b], lambda e: e.tensor_tensor(out=t0.t[:], in0=t0.t[:], in1=t1.t[:], op=ALU.add))
                kb.op('dve', [ups[2].b, g.b], [t1.b], lambda e: e.tensor_tensor(out=t1.t[:], in0=ups[2].t[:], in1=g.t[:, 2, cs], op=ALU.mult))
                kb.op('dve', [t0.b, t1.b], [mb.b], lambda e: e.tensor_tensor(out=mb.t[:, cs], in0=t0.t[:], in1=t1.t[:], op=ALU.add))
            for k in range(8):
                kb.op('pe', [mb.b, ident.b], [ptM.b], lambda e: e.transpose(out=ptM.t[:, k, :], in_=mb.t[:, k * 128:(k + 1) * 128], identity=ident.t[:]))
            kb.op('act', [ptM.b], [mT.b], lambda e: e.copy(out=mT.t[:], in_=ptM.t[:]))
            gt = gts[0] if t < 32 else gts[1]
            for half in range(2):
                cs = slice(half * 512, (half + 1) * 512)
                o2 = o2r.next()
                for kc in range(8):
                    kb.op('pe', [mT.b, wout.b], [o2.b], lambda e: e.matmul(o2.t[:], lhsT=mT.t[:, kc, :], rhs=wout.t[:, kc, cs], start=(kc == 0), stop=(kc == 7)))
                kb.op('dve', [o2.b, gt.b], [t0.b], lambda e: e.tensor_tensor(out=t0.t[:], in0=o2.t[:], in1=gt.t[:, cs], op=ALU.mult))
                kb.op('dve', [t0.b, xt.b], [xt.b], lambda e: e.tensor_tensor(out=xt.t[:, cs], in0=xt.t[:, cs], in1=t0.t[:], op=ALU.add))
            store(kb, xt, dr['xres'][rows, :], xt.t[:], eng='sp')


def top16(kb, src_tile, src_ap, work, vals_tile, vals_ap, idx_tile, idx_ap):
    kb.op('dve', [src_tile.b], [vals_tile.b], lambda e: e.max(out=vals_ap[:, 0:8], in_=src_ap))
    kb.op('dve', [src_tile.b, vals_tile.b], [idx_tile.b], lambda e: e.max_index(out=idx_ap[:, 0:8], in_max=vals_ap[:, 0:8], in_values=src_ap))
    kb.op('dve', [src_tile.b, vals_tile.b], [work.b], lambda e: e.match_replace(out=work.t[:, :src_ap.shape[-1]], in_to_replace=vals_ap[:, 0:8], in_values=src_ap, imm_value=-1e30))
    wk = work.t[:, :src_ap.shape[-1]]
    kb.op('dve', [work.b], [vals_tile.b], lambda e: e.max(out=vals_ap[:, 8:16], in_=wk))
    kb.op('dve', [work.b, vals_tile.b], [idx_tile.b], lambda e: e.max_index(out=idx_ap[:, 8:16], in_max=vals_ap[:, 8:16], in_values=wk))


def phase_peer(kb, dr, l, ident, ctx_out):
    last = (l == DEPTH - 1)
    ntile = NT if ctx_out else 32
    if CUT:
        ntile = 1
    with Phase(kb) as ph:
        stg = ph.ring(2, [128, 2048], F32, dma=True)
        wq = ph.sb([128, 8, 2048], BF16)
        load_bf16_weight(kb, ph, wq, 8, [dr['peer_wq'][l, i * 128:(i + 1) * 128, :] for i in range(8)], stg)
        keysT = ph.sb([128, 16, 128], BF16)
        ptk = ph.ps([128, 4, 128], BF16)
        for g4 in range(4):
            s_ = stg.next()
            load(kb, s_, s_.t[:, 0:512].rearrange("p (j d) -> p j d", j=4), dr['peer_keys'][l].rearrange("h p n d -> n (h p) d")[:, g4 * 4:(g4 + 1) * 4, :])
            kbf = ph.sb([128, 512], BF16)
            kb.op('dve', [s_.b], [kbf.b], lambda e: e.tensor_copy(out=kbf.t[:], in_=s_.t[:, 0:512]))
            for j in range(4):
                kb.op('pe', [kbf.b, ident.b], [ptk.b], lambda e: e.transpose(out=ptk.t[:, j, :], in_=kbf.t[:, j * 128:(j + 1) * 128], identity=ident.t[:]))
            kb.op('act', [ptk.b], [keysT.b], lambda e: e.copy(out=keysT.t[:, g4 * 4:(g4 + 1) * 4, :], in_=ptk.t[:]))
        AB = load_modAB(kb, ph, dr, l, 1, 'norm2_g')
        gts = []
        for r in range(2):
            g = ph.sb([128, D], F32, dma=True)
            load(kb, g, g.t[:], dr['mod'][l, r, 5 * D:6 * D].partition_broadcast(128))
            gts.append(g)
        iota = ph.sb([128, 16], F32, dma=True)
        load(kb, iota, iota.t[:], dr['iota16'])
        thr = ph.sb([128, 16], F32)
        kb.op('dve', [iota.b], [thr.b], lambda e: e.tensor_scalar(out=thr.t[:], in0=iota.t[:], scalar1=16.0, scalar2=None, op0=ALU.mult))
        if last:
            fg = ph.sb([128, D], F32, dma=True)
            load(kb, fg, fg.t[:], dr['final_g'].partition_broadcast(128))
        xr = ph.ring(2, [128, D], F32, dma=True)
        junk = ph.sb([128, D], F32); ss = ph.sb([128, 4], F32)
        h2f = ph.sb([128, D], F32); h2b = ph.sb([128, D], BF16); h2T = ph.sb([128, 8, 128], BF16)
        ptH = ph.ps([128, 8, 128], BF16)
        qpr = ph.ring(2, [128, 4, 128], F32, psum=True); scr = ph.ring(2, [128, 4, 128], F32, psum=True)
        qTb = ph.sb([128, 16, 128], BF16); S = ph.sb([128, 16, 128], F32)
        m16 = ph.sb([128, 16, 16], F32); i16 = ph.sb([128, 16, 16], U32); i16f = ph.sb([128, 16, 16], F32)
        work = ph.sb([128, 256], F32)
        cand = ph.sb([128, 8, 256], F32); eq = ph.sb([128, 8, 16, 16], F32)
        ts = ph.sb([128, 8, 16], F32); pos = ph.sb([128, 8, 16], U32); posf = ph.sb([128, 8, 16], F32)
        pa = ph.sb([128, 8, 16], F32); pb = ph.sb([128, 8, 16], F32)
        i1 = ph.sb([128, 8, 16], F32); i2 = ph.sb([128, 8, 16], F32)
        idx = ph.sb([128, 128], I32); gate = ph.sb([128, 8, 16], F32); gs = ph.sb([128, 8], F32)
        dots = ph.sb([128, 128], F32); wgt = ph.sb([128, 128], F32)
        acc = ph.sb([128, D], F32)
        slots = Ring([ph.sb([128, D], F32, dma=True, sw=True) for _ in range(8)])
        m16v = lambda: m16.t[:].rearrange("p (h two) k -> p h two k", two=2)
        i16v = lambda: i16f.t[:].rearrange("p (h two) k -> p h two k", two=2)
        for t in range(ntile):
            rows = slice(t * 128, (t + 1) * 128)
            xt = xr.next()
            load(kb, xt, xt.t[:], dr['xres'][rows, :])
            A, Bm = AB[0] if t < 32 else AB[1]
            rms_mod_tile(kb, ph, xt, A, Bm, h2f, h2f.t[:], (junk, ss))
            kb.op('act', [h2f.b], [h2b.b], lambda e: e.copy(out=h2b.t[:], in_=h2f.t[:]))
            for k in range(8):
                kb.op('pe', [h2b.b, ident.b], [ptH.b], lambda e: e.transpose(out=ptH.t[:, k, :], in_=h2b.t[:, k * 128:(k + 1) * 128], identity=ident.t[:]))
            kb.op('act', [ptH.b], [h2T.b], lambda e: e.copy(out=h2T.t[:], in_=ptH.t[:]))
            for g4 in range(4):
                qp = qpr.next()
                for j in range(4):
                    hp = g4 * 4 + j
                    for kc in range(8):
                        kb.op('pe', [wq.b, h2T.b], [qp.b], lambda e: e.matmul(qp.t[:, j, :], lhsT=wq.t[:, kc, hp * 128:(hp + 1) * 128], rhs=h2T.t[:, kc, :], start=(kc == 0), stop=(kc == 7)))
                kb.op('act', [qp.b], [qTb.b], lambda e: e.copy(out=qTb.t[:, g4 * 4:(g4 + 1) * 4, :], in_=qp.t[:]))
            for g4 in range(4):
                sc = scr.next()
                for j in range(4):
                    hp = g4 * 4 + j
                    kb.op('pe', [qTb.b, keysT.b], [sc.b], lambda e: e.matmul(sc.t[:, j, :], lhsT=qTb.t[:, hp, :], rhs=keysT.t[:, hp, :], start=True, stop=True))
                kb.op('act', [sc.b], [S.b], lambda e: e.copy(out=S.t[:, g4 * 4:(g4 + 1) * 4, :], in_=sc.t[:]))
            for hp in range(16):
                top16(kb, S, S.t[:, hp, :], work, m16, m16.t[:, hp, :], i16, i16.t[:, hp, :])
            kb.op('dve', [i16.b], [i16f.b], lambda e: e.tensor_copy(out=i16f.t[:], in_=i16.t[:]))
            kb.op('dve', [m16.b], [cand.b], lambda e: e.tensor_tensor(out=cand.t[:].rearrange("p h (a b) -> p h a b", a=16),
                  in0=m16v()[:, :, 0, :].unsqueeze(3).to_broadcast([128, 8, 16, 16]), in1=m16v()[:, :, 1, :].unsqueeze(2).to_broadcast([128, 8, 16, 16]), op=ALU.add))
            for h in range(8):
                top16(kb, cand, cand.t[:, h, :], work, ts, ts.t[:, h, :], pos, pos.t[:, h, :])
            kb.op('dve', [pos.b], [posf.b], lambda e: e.tensor_copy(out=posf.t[:], in_=pos.t[:]))
            kb.op('dve', [posf.b, thr.b], [eq.b], lambda e: e.tensor_tensor(out=eq.t[:], in0=posf.t[:].unsqueeze(3).to_broadcast([128, 8, 16, 16]), in1=thr.t[:].unsqueeze(1).unsqueeze(1).to_broadcast([128, 8, 16, 16]), op=ALU.is_ge))
            kb.op('dve', [eq.b], [pa.b], lambda e: e.reduce_sum(out=pa.t[:], in_=eq.t[:], axis=AX.X))
            kb.op('dve', [pa.b], [pa.b], lambda e: e.tensor_scalar(out=pa.t[:], in0=pa.t[:], scalar1=-1.0, scalar2=None, op0=ALU.add))
            kb.op('dve', [pa.b, posf.b], [pb.b], lambda e: e.scalar_tensor_tensor(out=pb.t[:], in0=pa.t[:], scalar=-16.0, in1=posf.t[:], op0=ALU.mult, op1=ALU.add))
            iob = iota.t[:].unsqueeze(1).unsqueeze(1).to_broadcast([128, 8, 16, 16])
            for (pp, two, dst) in ((pa, 0, i1), (pb, 1, i2)):
                kb.op('dve', [pp.b, iota.b], [eq.b], lambda e: e.tensor_tensor(out=eq.t[:], in0=pp.t[:].unsqueeze(3).to_broadcast([128, 8, 16, 16]), in1=iob, op=ALU.is_equal))
                kb.op('dve', [eq.b, i16f.b], [eq.b], lambda e: e.tensor_tensor(out=eq.t[:], in0=eq.t[:], in1=i16v()[:, :, two, :].unsqueeze(2).to_broadcast([128, 8, 16, 16]), op=ALU.mult))
                kb.op('dve', [eq.b], [dst.b], lambda e: e.reduce_sum(out=dst.t[:], in_=eq.t[:], axis=AX.X))
            kb.op('dve', [i1.b, i2.b], [i1.b], lambda e: e.scalar_tensor_tensor(out=i1.t[:], in0=i1.t[:], scalar=128.0, in1=i2.t[:], op0=ALU.mult, op1=ALU.add))
            kb.op('dve', [i1.b], [idx.b], lambda e: e.tensor_copy(out=idx.t[:], in_=i1.t[:].rearrange("p h k -> p (h k)")))
            kb.op('dve', [ts.b], [gate.b], lambda e: e.tensor_tensor(out=gate.t[:], in0=ts.t[:], in1=ts.t[:, :, 0:1].to_broadcast([128, 8, 16]), op=ALU.subtract))
            kb.op('act', [gate.b], [gate.b], lambda e: e.activation(out=gate.t[:], in_=gate.t[:], func=AF.Exp))
            kb.op('dve', [gate.b], [gs.b], lambda e: e.reduce_sum(out=gs.t[:], in_=gate.t[:], axis=AX.X))
            kb.op('dve', [gs.b], [gs.b], lambda e: e.reciprocal(out=gs.t[:], in_=gs.t[:]))
            kb.op('dve', [gate.b, gs.b], [gate.b], lambda e: e.tensor_tensor(out=gate.t[:], in0=gate.t[:], in1=gs.t[:].unsqueeze(2).to_broadcast([128, 8, 16]), op=ALU.mult))
            for r in range(128):
                sl = slots.next()
                kb.dma('pool', sl.ds, [idx.b], [sl.b], lambda e: e.indirect_dma_start(out=sl.t[:], out_offset=None, in_=dr[f'peer_u{l}'], in_offset=bass.IndirectOffsetOnAxis(ap=idx.t[:, r:r + 1], axis=0)))
                kb.op('dve', [sl.b, h2f.b], [junk.b, dots.b], lambda e: e.scalar_tensor_tensor(out=junk.t[:], in0=sl.t[:], scalar=1.0, in1=h2f.t[:], op0=ALU.mult, op1=ALU.mult, accum_out=dots.t[:, r:r + 1]))
            kb.op('act', [dots.b], [wgt.b], lambda e: e.activation(out=wgt.t[:], in_=dots.t[:], func=AF.Gelu))
            kb.op('dve', [wgt.b, gate.b], [wgt.b], lambda e: e.tensor_tensor(out=wgt.t[:], in0=wgt.t[:], in1=gate.t[:].rearrange("p h k -> p (h k)"), op=ALU.mult))
            for r in range(128):
                sl = slots.next()
                kb.dma('pool', sl.ds, [idx.b], [sl.b], lambda e: e.indirect_dma_start(out=sl.t[:], out_offset=None, in_=dr[f'peer_v{l}'], in_offset=bass.IndirectOffsetOnAxis(ap=idx.t[:, r:r + 1], axis=0)))
                if r == 0:
                    kb.op('dve', [sl.b, wgt.b], [acc.b], lambda e: e.tensor_scalar(out=acc.t[:], in0=sl.t[:], scalar1=wgt.t[:, 0:1], scalar2=None, op0=ALU.mult))
                else:
                    kb.op('dve', [sl.b, wgt.b, acc.b], [acc.b], lambda e: e.scalar_tensor_tensor(out=acc.t[:], in0=sl.t[:], scalar=wgt.t[:, r:r + 1], in1=acc.t[:], op0=ALU.mult, op1=ALU.add))
            gt = gts[0] if t < 32 else gts[1]
            kb.op('dve', [acc.b, gt.b], [acc.b], lambda e: e.tensor_tensor(out=acc.t[:], in0=acc.t[:], in1=gt.t[:], op=ALU.mult))
            kb.op('dve', [acc.b, xt.b], [xt.b], lambda e: e.tensor_tensor(out=xt.t[:], in0=xt.t[:], in1=acc.t[:], op=ALU.add))
            if not last:
                store(kb, xt, dr['xres'][rows, :], xt.t[:], eng='sp')
            else:
                if dr.get('dbg'):
                    store(kb, xt, dr['xres'][rows, :], xt.t[:], eng='sp')
                if t < 32:
                    rms_mod_tile_final(kb, xt, fg, (junk, ss))
                    store(kb, xt, dr['out'][rows, :], xt.t[:], eng='sp')


def rms_mod_tile_final(kb, xt, fg, scratch):
    junk, ss = scratch
    kb.op('act', [xt.b], [junk.b, ss.b], lambda e: e.activation(out=junk.t[:], in_=xt.t[:], func=AF.Square, accum_out=ss.t[:, 0:1]))
    kb.op('dve', [ss.b], [ss.b], lambda e: e.tensor_scalar(out=ss.t[:, 1:2], in0=ss.t[:, 0:1], scalar1=1.0 / D, scalar2=EPS, op0=ALU.mult, op1=ALU.add))
    kb.op('act', [ss.b], [ss.b], lambda e: e.sqrt(out=ss.t[:, 2:3], in_=ss.t[:, 1:2]))
    kb.op('dve', [ss.b], [ss.b], lambda e: e.reciprocal(out=ss.t[:, 3:4], in_=ss.t[:, 2:3]))
    kb.op('dve', [xt.b, ss.b, fg.b], [xt.b], lambda e: e.scalar_tensor_tensor(out=xt.t[:], in0=xt.t[:], scalar=ss.t[:, 3:4], in1=fg.t[:], op0=ALU.mult, op1=ALU.mult))


def w_ext_perm():
    a = np.arange
    o = {'aq': 0, 'ak': 512, 'av': 1024, 'az': 1536, 'b': 2048, 'a': 2056, 'bq': 2064, 'bk': 2576, 'bv': 3088,
         'cq': 3600, 'ck': 4112, 'cv': 4624, 'g': 5136}
    def partner(base):
        idx = []
        for h in range(4):
            for m in range(2):
                for dd in range(64):
                    seg = dd // 32; j = dd % 32
                    pj = j + 16 if j < 16 else j - 16
                    idx.append(base + h * 128 + m * 64 + seg * 32 + pj)
        return np.array(idx)
    cols = np.concatenate([a(0, 2048), a(o['bq'], o['bq'] + 1536), a(o['cq'], o['cq'] + 1536), a(o['g'], o['g'] + 3072),
                           partner(o['bq']), partner(o['bk']), a(2048, 2064)])
    assert cols.shape[0] == NP
    return cols


def build(dbg=False, stop_after=None):
    nc = bass.Bass("TRN2", target_bir_lowering=False)
    dr = {}
    def din(name, shape, dt=F32):
        dr[name] = nc.dram_tensor(name, list(shape), dt, kind="ExternalInput").ap()
    def dscr(name, shape, dt=F32):
        dr[name] = nc.dram_tensor(name, list(shape), dt, kind="ExternalOutput" if dbg else "Internal").ap()
    din('x', [L, D]); din('ctx', [LC, D]); din('c', [D]); din('c_ctx', [D])
    din('norm1_g', [DEPTH, D]); din('norm2_g', [DEPTH, D]); din('ada_w', [DEPTH, D, 6 * D]); din('ada_b', [DEPTH, 6 * D])
    din('w_ext', [DEPTH, D, NP]); din('ident', [128, 128])
    din('rope', [T, 128]); din('diff_lambda', [DEPTH, 4, 64]); din('diff_subln_g', [DEPTH, 128])
    din('na_mask', [64, 64]); din('rpbg', [DEPTH, 8, 64, 15, 64])
    din('gdn_conv', [DEPTH, 5, 1536]); din('gdn_a_log', [DEPTH, 2, 4]); din('gdn_dt_bias', [DEPTH, 2, 4]); din('gdn_norm_g', [DEPTH, 128]); din('gconst', [64, 5, 64])
    din('w_up', [DEPTH, 3, 512, D]); din('w_out', [DEPTH, D, D]); din('peer_wq', [DEPTH, D, 2048]); din('peer_keys', [DEPTH, 8, 2, 128, 128])
    if 'peer' not in SKIP:
        for l_ in range(DEPTH):
            din(f'peer_u{l_}', [16384, D]); din(f'peer_v{l_}', [16384, D])
    din('final_g', [D]); din('iota16', [128, 16])
    if dbg:
        dr['dbg'] = True
    dscr('mod', [DEPTH, 2, 6 * D]); dscr('proj', [T, NP]); dscr('xres', [T, D]); dscr('ybr', [T, 1536])
    dscr('gqkv', [T, 1536]); dscr('gbg', [T, 16])
    dr['out'] = nc.dram_tensor('out', [L, D], F32, kind="ExternalOutput").ap()
    with ExitStack() as es:
        kb = KB(nc, es)
        idf = Tile(kb, es.enter_context(nc.sbuf_tensor('idf', [128, 128], F32)), kb.dsem())
        ident = Tile(kb, es.enter_context(nc.sbuf_tensor('idb', [128, 128], BF16)))
        load(kb, idf, idf.t[:], dr['ident'])
        kb.op('dve', [idf.b], [ident.b], lambda e: e.tensor_copy(out=ident.t[:], in_=idf.t[:]))
        for l in range(DEPTH):
            if 'mod' not in SKIP:
                phase_mod(kb, dr, l)
            if stop_after == ('mod', l): break
            if 'inproj' not in SKIP:
                phase_inproj(kb, dr, l, ident)
            if stop_after == ('inproj', l): break
            ctx_out = l < DEPTH - 1
            if 'gdn' not in SKIP:
                phase_gdn_prep(kb, dr, l)
                if stop_after == ('gdnprep', l): break
                phase_gdn(kb, dr, l, ident)
            if stop_after == ('gdn', l): break
            if 'diff' not in SKIP:
                phase_diff(kb, dr, l, ident, ctx_out)
            if stop_after == ('diff', l): break
            if 'natten' not in SKIP:
                phase_natten(kb, dr, l, ident, ctx_out)
            if stop_after == ('natten', l): break
            if 'merge' not in SKIP:
                phase_merge(kb, dr, l, ident, ctx_out)
            if stop_after == ('merge', l): break
            if 'peer' not in SKIP:
                phase_peer(kb, dr, l, ident, ctx_out)
            if stop_after == ('peer', l): break
        kb.barrier()
        print('instructions:', kb.ninst)
    return nc


def host_consts():
    t = np.arange(L)
    row, colp = t // 64, t % 64
    inv = 1.0 / (10000.0 ** (np.arange(16, dtype=np.float32) / 16))
    cos = np.ones((T, 64), np.float32); sin = np.zeros((T, 64), np.float32)
    for seg, pos in ((0, row), (1, colp)):
        ang = pos.astype(np.float32)[:, None] * inv[None, :]
        c, s_ = np.cos(ang), np.sin(ang)
        cos[:L, seg * 32:seg * 32 + 16] = c; cos[:L, seg * 32 + 16:seg * 32 + 32] = c
        sin[:L, seg * 32:seg * 32 + 16] = -s_; sin[:L, seg * 32 + 16:seg * 32 + 32] = s_
    col = np.arange(64)
    cs = np.clip(col - 8, 0, 48)
    ok = (col[None, :] >= cs[:, None]) & (col[None, :] < cs[:, None] + 16)
    mask = np.where(ok.T, 0.0, -30000.0).astype(np.float32)
    ii = np.arange(64)
    Lf = (ii[:, None] <= ii[None, :]).astype(np.float32)
    Lb = (ii[:, None] >= ii[None, :]).astype(np.float32)
    Sf = (ii[None, :] < ii[:, None]).astype(np.float32)
    Sb_ = (ii[None, :] > ii[:, None]).astype(np.float32)
    gconst = np.stack([Lf, Lb, Sf, Sb_, np.eye(64, dtype=np.float32)], axis=1)
    return {'rope': np.concatenate([cos, sin], 1).astype(np.float32), 'na_mask': mask, 'gconst': np.ascontiguousarray(gconst)}


def make_in_maps(inputs):
    perm = w_ext_perm()
    w_ext = np.ascontiguousarray(inputs['w_in'][:, :, perm])
    shared = {k: np.ascontiguousarray(inputs[k]) for k in ('c_ctx', 'norm1_g', 'norm2_g', 'ada_w', 'ada_b')}
    shared['w_ext'] = w_ext
    shared['ident'] = np.eye(128, dtype=np.float32)
    shared.update(host_consts())
    shared['iota16'] = np.tile(np.arange(16, dtype=np.float32)[None, :], (128, 1))
    for k in ('gdn_conv', 'gdn_a_log', 'gdn_dt_bias', 'gdn_norm_g', 'diff_lambda', 'diff_subln_g', 'w_up', 'w_out', 'peer_wq', 'peer_keys', 'final_g'):
        shared[k] = np.ascontiguousarray(inputs[k])
    for l_ in range(DEPTH):
        shared[f'peer_u{l_}'] = np.ascontiguousarray(inputs['peer_u'][l_]); shared[f'peer_v{l_}'] = np.ascontiguousarray(inputs['peer_v'][l_])
    col = np.arange(64)
    dc = np.clip(col[None, :] - col[:, None], -15, 15) + 15
    rp = inputs['na_rpb']
    shared['rpbg'] = np.ascontiguousarray(rp[:, :, :, dc.T].transpose(0, 1, 3, 2, 4))
    maps = []
    for c in range(8):
        b = c % 4
        m = dict(shared)
        m['x'] = np.ascontiguousarray(inputs['x'][b]); m['ctx'] = np.ascontiguousarray(inputs['ctx'][b])
        m['c'] = np.ascontiguousarray(inputs['c'][b])
        maps.append(m)
    return maps


def kernel(**inputs):
    inputs = {k: np.asarray(v) for k, v in inputs.items()}
    nc = build()
    maps = make_in_maps(inputs)
    if 'peer' in SKIP:
        maps = [{k: v for k, v in m.items() if not k.startswith(('peer_u', 'peer_v'))} for m in maps]
    res = run_bass_kernel_spmd(nc, maps, core_ids=list(range(8)))
    return np.stack([res.results[b]['out'] for b in range(4)], axis=0)
```
